# Optimizing a Trainium2 kernel written in Bass

```python
import jax, jax.numpy as jnp
from jax import lax
import numpy as np

D_MODEL = 4096
BATCH = 2
SEQ = 8192
DEPTH = 2

GRID_W = 64
CTX_LEN = 256
HEAD_DIM = 128
ROPE_THETA = 10000.0
NORM_EPS = 1e-6
Q_BLOCK = 128
N_MOD = 6
N_BRANCH = 4
BRANCH_WIDTH = D_MODEL // N_BRANCH
MLA_HEADS = BRANCH_WIDTH // HEAD_DIM
MLA_Q_LORA = 1024
MLA_KV_LORA = 512
MLA_NOPE = 128
MLA_ROPE = 64
MLA_V = HEAD_DIM
GQA_HEADS = BRANCH_WIDTH // HEAD_DIM
GQA_KV_HEADS = 2
NA_HEADS = BRANCH_WIDTH // HEAD_DIM
NA_WIN_H = 8
NA_WIN_W = 16
LRU_WIDTH = BRANCH_WIDTH
LRU_BLOCKS = 8
LRU_CONV = 4
LRU_C = 8.0
N_EXPERTS = 16
N_GROUPS = 4
TOP_K = 2
D_EXPERT = 1024

ATTN_SCALE = HEAD_DIM ** -0.5
MLA_SCALE = (MLA_NOPE + MLA_ROPE) ** -0.5
IN_SPLITS = (MLA_Q_LORA, MLA_KV_LORA, MLA_ROPE,
             GQA_HEADS * HEAD_DIM, GQA_KV_HEADS * HEAD_DIM, GQA_KV_HEADS * HEAD_DIM,
             NA_HEADS * HEAD_DIM, NA_HEADS * HEAD_DIM, NA_HEADS * HEAD_DIM,
             LRU_WIDTH, LRU_WIDTH)
D_IN = sum(IN_SPLITS)

kernel_name = 'hybrid_dit_mla_gqa_natten_rglru_moe'

F32 = jnp.float32


def rmsnorm(x, g):
    xf = x.astype(F32)
    y = xf * lax.rsqrt(jnp.mean(xf * xf, axis=-1, keepdims=True) + NORM_EPS)
    return (y * g.astype(F32)).astype(x.dtype)


def modulate(x, g, shift, scale):
    return rmsnorm(x, g) * (1.0 + scale) + shift


def heads(x, n):
    return x.reshape(x.shape[:-1] + (n, HEAD_DIM))


def axial_rope_tables(n_tokens, rot_dim):
    t = jnp.arange(n_tokens, dtype=jnp.int32)
    row = (t // GRID_W).astype(F32)
    col = (t % GRID_W).astype(F32)
    n_freq = rot_dim // 4
    freqs = ROPE_THETA ** (-jnp.arange(n_freq, dtype=F32) / n_freq)
    ang = jnp.concatenate([row[:, None] * freqs, col[:, None] * freqs], axis=-1)
    return jnp.cos(ang), jnp.sin(ang)


def apply_rope(x, cos, sin):
    half = x.shape[-1] // 2
    x1 = x[..., :half].astype(F32)
    x2 = x[..., half:].astype(F32)
    cs, sn = cos[:, None, :], sin[:, None, :]
    return jnp.concatenate([x1 * cs - x2 * sn, x1 * sn + x2 * cs], axis=-1).astype(x.dtype)


def blocked_attention(q, k, v, scale):
    B, S, H, Dk = q.shape
    Hk = k.shape[2]
    G = H // Hk
    nb = S // Q_BLOCK
    qb = q.reshape(B, nb, Q_BLOCK, Hk, G, Dk).transpose(1, 0, 2, 3, 4, 5)

    def one_block(qblk):
        s = jnp.einsum('bqkgd,btkd->bkgqt', qblk, k).astype(F32) * scale
        p = jax.nn.softmax(s, axis=-1).astype(v.dtype)
        return jnp.einsum('bkgqt,btkd->bqkgd', p, v)

    out = lax.map(one_block, qb)
    return out.transpose(1, 0, 2, 3, 4, 5).reshape(B, S, H * v.shape[-1])


def mla_qkv(cq, ckv, kr, q_norm, w_uq, kv_norm, w_ukv, rope):
    B, T, _ = cq.shape
    q = (rmsnorm(cq, q_norm) @ w_uq).reshape(B, T, MLA_HEADS, MLA_NOPE + MLA_ROPE)
    kv = (rmsnorm(ckv, kv_norm) @ w_ukv).reshape(B, T, MLA_HEADS, MLA_NOPE + MLA_V)
    q_nope, q_rope = q[..., :MLA_NOPE], q[..., MLA_NOPE:]
    k_nope, v = kv[..., :MLA_NOPE], kv[..., MLA_NOPE:]
    kr = kr[:, :, None, :]
    if rope is not None:
        q_rope = apply_rope(q_rope, rope[0], rope[1])
        kr = apply_rope(kr, rope[0], rope[1])
    q = jnp.concatenate([q_nope, q_rope], axis=-1)
    k = jnp.concatenate([k_nope, jnp.broadcast_to(kr, (B, T, MLA_HEADS, MLA_ROPE))], axis=-1)
    return q, k, v


def neighbourhood_attention(q, k, v, k_ctx, v_ctx, rpb):
    B, S, H, Dh = q.shape
    rows = S // GRID_W
    kh = min(NA_WIN_H, rows)
    kw = NA_WIN_W
    qg = q.reshape(B, rows, GRID_W, H, Dh).transpose(1, 0, 2, 3, 4)
    kg = k.reshape(B, rows, GRID_W, H, Dh)
    vg = v.reshape(B, rows, GRID_W, H, Dh)
    row_ids = jnp.arange(rows, dtype=jnp.int32)
    row_start = jnp.clip(row_ids - kh // 2, 0, rows - kh)
    col_ids = np.arange(GRID_W)
    col_start = np.clip(col_ids - kw // 2, 0, GRID_W - kw)
    col_idx = col_start[:, None] + np.arange(kw)[None, :]
    dc_idx = col_idx - col_ids[:, None] + (NA_WIN_W - 1)
    rpb_cols = rpb[:, :, dc_idx]

    def one_row(args):
        qb, r, rs = args
        kb = lax.dynamic_slice_in_dim(kg, rs, kh, axis=1)[:, :, col_idx]
        vb = lax.dynamic_slice_in_dim(vg, rs, kh, axis=1)[:, :, col_idx]
        dr_idx = rs + jnp.arange(kh, dtype=jnp.int32) - r + (NA_WIN_H - 1)
        bias = rpb_cols[:, dr_idx].transpose(0, 2, 1, 3)
        s_loc = jnp.einsum('bqhd,biqjhd->bhqij', qb, kb).astype(F32) * ATTN_SCALE + bias.astype(F32)
        s_ctx = jnp.einsum('bqhd,bthd->bhqt', qb, k_ctx).astype(F32) * ATTN_SCALE
        s = jnp.concatenate([s_loc.reshape(B, H, GRID_W, kh * kw), s_ctx], axis=-1)
        p = jax.nn.softmax(s, axis=-1).astype(v.dtype)
        p_loc = p[..., :kh * kw].reshape(B, H, GRID_W, kh, kw)
        p_ctx = p[..., kh * kw:]
        return (jnp.einsum('bhqij,biqjhd->bqhd', p_loc, vb)
                + jnp.einsum('bhqt,bthd->bqhd', p_ctx, v_ctx))

    out = lax.map(one_row, (qg, row_ids, row_start))
    return out.transpose(1, 0, 2, 3, 4).reshape(B, S, H * Dh)


def centred_dwconv(x, w, b):
    y = lax.conv_general_dilated(
        x, w[:, None, :], window_strides=(1,),
        padding=[(LRU_CONV // 2, LRU_CONV - 1 - LRU_CONV // 2)],
        dimension_numbers=('NWC', 'WIO', 'NWC'), feature_group_count=x.shape[-1])
    return y + b


def _lin_combine(e1, e2):
    a1, b1 = e1
    a2, b2 = e2
    return a1 * a2, a2 * b1 + b2


def rglru_scan(xc, w_a, b_a, w_x, b_x, lam, h0, reverse):
    B, T, W = xc.shape
    xb = xc.reshape(B, T, LRU_BLOCKS, W // LRU_BLOCKS)
    r = jax.nn.sigmoid((jnp.einsum('btnc,ncd->btnd', xb, w_a).reshape(B, T, W) + b_a).astype(F32))
    i = jax.nn.sigmoid((jnp.einsum('btnc,ncd->btnd', xb, w_x).reshape(B, T, W) + b_x).astype(F32))
    log_a = -LRU_C * r * jax.nn.softplus(-lam.astype(F32))
    a = jnp.exp(log_a)
    bterm = jnp.sqrt(-jnp.expm1(2.0 * log_a)) * (i * xc.astype(F32))
    edge = T - 1 if reverse else 0
    bterm = bterm.at[:, edge].add(a[:, edge] * h0)
    _, h = lax.associative_scan(_lin_combine, (a, bterm), axis=1, reverse=reverse)
    return h


def rglru_branch(x_lat, g_lat, x_ctx, g_ctx, conv_w, conv_b, w_a, b_a, w_x, b_x, lam, need_ctx):
    xl = centred_dwconv(x_lat, conv_w, conv_b)
    xc = centred_dwconv(x_ctx, conv_w, conv_b)
    B = x_ctx.shape[0]
    h0 = jnp.zeros((B, LRU_WIDTH), F32)
    hc_f = rglru_scan(xc, w_a[0], b_a[0], w_x[0], b_x[0], lam[0], h0, False)
    hl_f = rglru_scan(xl, w_a[0], b_a[0], w_x[0], b_x[0], lam[0], hc_f[:, -1], False)
    hc_b = rglru_scan(xc, w_a[1], b_a[1], w_x[1], b_x[1], lam[1], h0, True)
    hl_b = rglru_scan(xl, w_a[1], b_a[1], w_x[1], b_x[1], lam[1], hc_b[:, 0], True)
    y_lat = ((hl_f + hl_b) * jax.nn.gelu(g_lat.astype(F32))).astype(x_lat.dtype)
    y_ctx = ((hc_f + hc_b) * jax.nn.gelu(g_ctx.astype(F32))).astype(x_ctx.dtype) if need_ctx else None
    return y_lat, y_ctx


def merge_branches(h, ys, w_branch_gate, b_branch_gate, w_branch, w_out):
    merged = None
    for i, y in enumerate(ys):
        g = jax.nn.sigmoid(h @ w_branch_gate[i] + b_branch_gate[i])
        term = g * (y @ w_branch[i])
        merged = term if merged is None else merged + term
    return merged @ w_out


def split_in(p):
    offs = np.cumsum(np.array(IN_SPLITS))[:-1].tolist()
    return jnp.split(p, offs, axis=-1)


def token_mixers(h_lat, h_ctx, need_ctx, w_in, mla_q_norm, mla_w_uq, mla_kv_norm, mla_w_ukv,
                 gqa_q_norm, gqa_k_norm, na_rpb, lru_conv_w, lru_conv_b, lru_w_a, lru_b_a,
                 lru_w_x, lru_b_x, lru_lambda, w_branch_gate, b_branch_gate, w_branch, w_out,
                 rope128, rope64):
    B, S, _ = h_lat.shape
    pl = split_in(h_lat @ w_in)
    pc = split_in(h_ctx @ w_in)

    qa, ka, va = mla_qkv(pl[0], pl[1], pl[2], mla_q_norm, mla_w_uq, mla_kv_norm, mla_w_ukv, rope64)
    qac, kac, vac = mla_qkv(pc[0], pc[1], pc[2], mla_q_norm, mla_w_uq, mla_kv_norm, mla_w_ukv, None)
    ya = blocked_attention(qa, jnp.concatenate([kac, ka], 1), jnp.concatenate([vac, va], 1), MLA_SCALE)

    qb = apply_rope(rmsnorm(heads(pl[3], GQA_HEADS), gqa_q_norm), rope128[0], rope128[1])
    kb = apply_rope(rmsnorm(heads(pl[4], GQA_KV_HEADS), gqa_k_norm), rope128[0], rope128[1])
    vb = heads(pl[5], GQA_KV_HEADS)
    qbc = rmsnorm(heads(pc[3], GQA_HEADS), gqa_q_norm)
    kbc = rmsnorm(heads(pc[4], GQA_KV_HEADS), gqa_k_norm)
    vbc = heads(pc[5], GQA_KV_HEADS)
    yb = blocked_attention(qb, jnp.concatenate([kbc, kb], 1), jnp.concatenate([vbc, vb], 1), ATTN_SCALE)

    kcc, vcc = heads(pc[7], NA_HEADS), heads(pc[8], NA_HEADS)
    yc = neighbourhood_attention(heads(pl[6], NA_HEADS), heads(pl[7], NA_HEADS), heads(pl[8], NA_HEADS),
                                 kcc, vcc, na_rpb)

    yd, ydc = rglru_branch(pl[9], pl[10], pc[9], pc[10], lru_conv_w, lru_conv_b, lru_w_a, lru_b_a,
                           lru_w_x, lru_b_x, lru_lambda, need_ctx)

    y_lat = merge_branches(h_lat, (ya, yb, yc, yd), w_branch_gate, b_branch_gate, w_branch, w_out)
    if not need_ctx:
        return y_lat, None
    yac = blocked_attention(qac, kac, vac, MLA_SCALE)
    ybc = blocked_attention(qbc, kbc, vbc, ATTN_SCALE)
    ycc = blocked_attention(heads(pc[6], NA_HEADS), kcc, vcc, ATTN_SCALE)
    y_ctx = merge_branches(h_ctx, (yac, ybc, ycc, ydc), w_branch_gate, b_branch_gate, w_branch, w_out)
    return y_lat, y_ctx


def moe_ffn(h, w_router, router_bias, w_gate, w_up, w_down):
    scores = jax.nn.sigmoid((h @ w_router).astype(F32))
    sel = scores + router_bias.astype(F32)
    per = N_EXPERTS // N_GROUPS
    group_score = lax.top_k(sel.reshape(-1, N_GROUPS, per), 2)[0].sum(-1)
    best = jnp.argmax(group_score, axis=-1)
    expert_group = jnp.arange(N_EXPERTS, dtype=jnp.int32) // per
    masked = jnp.where(expert_group[None, :] == best[:, None], sel, -jnp.inf)
    _, idx = lax.top_k(masked, TOP_K)
    wts = jnp.take_along_axis(scores, idx, axis=-1)
    wts = wts / jnp.sum(wts, axis=-1, keepdims=True)
    gates = jnp.einsum('nk,nke->ne', wts, jax.nn.one_hot(idx, N_EXPERTS, dtype=F32)).astype(h.dtype)
    y = None
    for e in range(N_EXPERTS):
        he = jax.nn.silu(h @ w_gate[e]) * (h @ w_up[e])
        term = gates[:, e:e + 1] * (he @ w_down[e])
        y = term if y is None else y + term
    return y


def setup_inputs(seed: int = 0) -> dict:
    key = jax.random.key(seed)
    ks = jax.random.split(key, 48)
    D, L = D_MODEL, DEPTH

    def nrm(i, shape, scale):
        return jax.random.normal(ks[i], shape, F32) * scale

    def gain(i, shape):
        return 1.0 + 0.02 * jax.random.normal(ks[i], shape, F32)

    a0 = jax.random.uniform(ks[40], (L, 2, LRU_WIDTH), F32, 0.9, 0.999) ** (1.0 / LRU_C)
    lam = jnp.log(a0) - jnp.log1p(-a0)
    bw = LRU_WIDTH // LRU_BLOCKS
    return {
        'x': nrm(0, (BATCH, SEQ, D), 1.0),
        'c': nrm(1, (BATCH, D), 1.0),
        'ctx': nrm(2, (BATCH, CTX_LEN, D), 1.0),
        'c_ctx': nrm(3, (D,), 1.0),
        'w_mod': nrm(4, (L, D, N_MOD * D), 0.5 * D ** -0.5),
        'b_mod': nrm(5, (L, N_MOD * D), 0.02),
        'norm_mix': gain(6, (L, D)),
        'norm_ffn': gain(7, (L, D)),
        'w_in': nrm(8, (L, D, D_IN), D ** -0.5),
        'mla_q_norm': gain(9, (L, MLA_Q_LORA)),
        'mla_w_uq': nrm(10, (L, MLA_Q_LORA, MLA_HEADS * (MLA_NOPE + MLA_ROPE)), MLA_Q_LORA ** -0.5),
        'mla_kv_norm': gain(11, (L, MLA_KV_LORA)),
        'mla_w_ukv': nrm(12, (L, MLA_KV_LORA, MLA_HEADS * (MLA_NOPE + MLA_V)), MLA_KV_LORA ** -0.5),
        'gqa_q_norm': gain(13, (L, HEAD_DIM)),
        'gqa_k_norm': gain(14, (L, HEAD_DIM)),
        'na_rpb': nrm(15, (L, NA_HEADS, 2 * NA_WIN_H - 1, 2 * NA_WIN_W - 1), 0.1),
        'lru_conv_w': nrm(16, (L, LRU_CONV, LRU_WIDTH), 0.5),
        'lru_conv_b': nrm(17, (L, LRU_WIDTH), 0.02),
        'lru_w_a': nrm(18, (L, 2, LRU_BLOCKS, bw, bw), bw ** -0.5),
        'lru_b_a': nrm(19, (L, 2, LRU_WIDTH), 0.02),
        'lru_w_x': nrm(20, (L, 2, LRU_BLOCKS, bw, bw), bw ** -0.5),
        'lru_b_x': nrm(21, (L, 2, LRU_WIDTH), 0.02),
        'lru_lambda': lam,
        'w_branch_gate': nrm(22, (L, N_BRANCH, D, D), D ** -0.5),
        'b_branch_gate': nrm(23, (L, N_BRANCH, D), 0.02),
        'w_branch': nrm(24, (L, N_BRANCH, BRANCH_WIDTH, D), BRANCH_WIDTH ** -0.5),
        'w_out': nrm(25, (L, D, D), D ** -0.5),
        'w_router': nrm(26, (D, N_EXPERTS), D ** -0.5),
        'router_bias': nrm(27, (N_EXPERTS,), 0.01),
        'w_exp_gate': nrm(28, (L, N_EXPERTS, D, D_EXPERT), D ** -0.5),
        'w_exp_up': nrm(29, (L, N_EXPERTS, D, D_EXPERT), D ** -0.5),
        'w_exp_down': nrm(30, (L, N_EXPERTS, D_EXPERT, D), D_EXPERT ** -0.5),
        'final_norm': gain(31, (D,)),
    }


def reference(x, c, ctx, c_ctx, w_mod, b_mod, norm_mix, norm_ffn, w_in, mla_q_norm, mla_w_uq,
              mla_kv_norm, mla_w_ukv, gqa_q_norm, gqa_k_norm, na_rpb, lru_conv_w, lru_conv_b,
              lru_w_a, lru_b_a, lru_w_x, lru_b_x, lru_lambda, w_branch_gate, b_branch_gate,
              w_branch, w_out, w_router, router_bias, w_exp_gate, w_exp_up, w_exp_down, final_norm):
    B, S, D = x.shape
    rope128 = axial_rope_tables(S, HEAD_DIM)
    rope64 = axial_rope_tables(S, MLA_ROPE)
    silu_c = jax.nn.silu(c)
    silu_cc = jax.nn.silu(c_ctx)
    for layer in range(DEPTH):
        need_ctx = layer < DEPTH - 1
        mod = (silu_c @ w_mod[layer] + b_mod[layer]).reshape(B, N_MOD, 1, D)
        mod_c = (silu_cc @ w_mod[layer] + b_mod[layer]).reshape(N_MOD, D)
        h = modulate(x, norm_mix[layer], mod[:, 0], mod[:, 1])
        hc = modulate(ctx, norm_mix[layer], mod_c[0], mod_c[1])
        y, yc = token_mixers(h, hc, need_ctx, w_in[layer], mla_q_norm[layer], mla_w_uq[layer],
                             mla_kv_norm[layer], mla_w_ukv[layer], gqa_q_norm[layer], gqa_k_norm[layer],
                             na_rpb[layer], lru_conv_w[layer], lru_conv_b[layer], lru_w_a[layer],
                             lru_b_a[layer], lru_w_x[layer], lru_b_x[layer], lru_lambda[layer],
                             w_branch_gate[layer], b_branch_gate[layer], w_branch[layer], w_out[layer],
                             rope128, rope64)
        x = x + mod[:, 2] * y
        h2 = modulate(x, norm_ffn[layer], mod[:, 3], mod[:, 4]).reshape(B * S, D)
        if need_ctx:
            ctx = ctx + mod_c[2] * yc
            h2c = modulate(ctx, norm_ffn[layer], mod_c[3], mod_c[4]).reshape(-1, D)
            f = moe_ffn(jnp.concatenate([h2, h2c], axis=0), w_router, router_bias,
                        w_exp_gate[layer], w_exp_up[layer], w_exp_down[layer])
            x = x + mod[:, 5] * f[:B * S].reshape(B, S, D)
            ctx = ctx + mod_c[5] * f[B * S:].reshape(ctx.shape)
        else:
            f = moe_ffn(h2, w_router, router_bias, w_exp_gate[layer], w_exp_up[layer], w_exp_down[layer])
            x = x + mod[:, 5] * f.reshape(B, S, D)
    return rmsnorm(x, final_norm)
```

```python
import numpy as np
from contextlib import ExitStack
import concourse.bass as bass
import concourse.mybir as mybir
from concourse.bass_utils import run_bass_kernel_spmd

F32 = mybir.dt.float32
BF16 = mybir.dt.bfloat16
AF = mybir.ActivationFunctionType
ALU = mybir.AluOpType
AX = mybir.AxisListType

FULL = dict(D=4096, B=2, S=8192, CTX=256, CPB=4, QL=1024, KVL=512, DE=1024, NE=16, DEPTH=2,
            GRID_W=64, WH=8, WW=16)
CC_INC = 1
DBG_LEVEL = 99
FAKE_CC = False
NEG = -30000.0


def derive(c):
    c = dict(c)
    c['BW'] = c['D'] // 4
    c['NH'] = c['BW'] // 128
    c['HPC'] = c['NH'] // c['CPB']
    c['KC'] = c['D'] // 128
    c['NCORE'] = c['B'] * c['CPB']
    c['TL'] = c['S'] // c['CPB']
    c['TC'] = c['CTX'] // c['CPB']
    c['TOK'] = c['TL'] + c['TC']
    c['TB'] = c['S'] + c['CTX']
    c['G'] = c['NH'] // 2
    c['MS'] = 6 * c['D'] // c['NCORE']
    H = c['HPC']
    segs = [('cq', c['QL']), ('ckv', c['KVL']), ('kr', 64), ('krs', 64), ('gq', H * 128), ('gqs', H * 128),
            ('gk', 128), ('gks', 128), ('nq', H * 128), ('nk', H * 128),
            ('lx', H * 128), ('lg', H * 128), ('gv', 128), ('nv', H * 128)]
    off = {}
    o = 0
    for n, w in segs:
        off[n] = (o, w)
        o += w
    c['SEG'] = off
    c['NCOL'] = o
    return c


class Op:
    __slots__ = ('eng', 'fn', 'r', 'w', 'dma', 'deps', 'ms', 'need', 'sem', 'val', 'prev', 'inc')


class Stage:
    NSEM = {'sp': 8, 'pool': 6, 'act': 4, 'cc': 2}
    SEMS = None
    DVAL = {}

    def __init__(self, nc, name):
        self.nc = nc
        self.name = name
        self.ops = []
        self.es = ExitStack()
        self.cnt = 0
        self.npsum = 0

    def sb(self, shape, dt, name=None):
        self.cnt += 1
        return self.es.enter_context(self.nc.sbuf_tensor(f"{self.name}_{name or 't'}{self.cnt}", list(shape), dt))

    def ps(self, name=None, shape=(128, 512), dt=F32):
        self.cnt += 1
        return self.es.enter_context(self.nc.psum_tensor(f"{self.name}_{name or 'p'}{self.cnt}", list(shape), dt))

    def add(self, eng, fn, r=(), w=(), dma=False, inc=16):
        o = Op()
        o.eng = eng; o.fn = fn; o.r = tuple(r); o.w = tuple(w); o.dma = dma; o.need = False
        o.ms = 0; o.sem = None; o.val = 0; o.prev = 0; o.inc = inc
        self.ops.append(o)
        return o

    def mm(self, out, lhsT, rhs, start, stop, r, w):
        self.add('pe', lambda e: e.matmul(out, lhsT, rhs, start=start, stop=stop), r, w)

    def tr(self, out, in_, ident, r, w):
        self.add('pe', lambda e: e.transpose(out, in_, ident), r, w)

    def act(self, out, in_, func, r, w, bias=0.0, scale=1.0):
        self.add('act', lambda e: e.activation(out, in_, func, bias=bias, scale=scale), r, w)

    def dve(self, fn, r, w):
        self.add('dve', fn, r, w)

    def pool(self, fn, r, w):
        self.add('pool', fn, r, w)

    def dma(self, out, in_, r, w, q='sp', **kw):
        self.add(q, lambda e: e.dma_start(out=out, in_=in_, **kw), r, w, dma=True)

    def cc(self, kind, groups, in_ap, out_ap, r, w):
        op = ALU.bypass
        if FAKE_CC:
            n = len(groups[0])
            R = in_ap.shape[0]
            for i in range(n):
                self.dma(out_ap[i * R:(i + 1) * R, :], in_ap, r=r, w=w)
            return
        self.add('pool', lambda e: e.collective_compute(kind, op, replica_groups=groups, ins=[in_ap], outs=[out_ap]),
                 r, w, dma=True, inc=CC_INC)

    def run(self):
        nc = self.nc
        ops = self.ops
        last_w = {}
        rd_eng = {}
        rd_dma = {}
        for i, op in enumerate(ops):
            deps = set()
            for k in op.r:
                j = last_w.get(k)
                if j is not None:
                    deps.add(j)
            for k in op.w:
                j = last_w.get(k)
                if j is not None:
                    deps.add(j)
                d = rd_eng.get(k)
                if d:
                    deps.update(d.values())
                d = rd_dma.get(k)
                if d:
                    deps.update(d)
            deps.discard(i)
            fd = []
            for j in deps:
                pj = ops[j]
                if (not pj.dma) and (not op.dma) and pj.eng == op.eng == 'pe':
                    continue
                fd.append(j)
                if not pj.dma:
                    pj.need = True
            op.deps = fd
            for k in op.w:
                last_w[k] = i
                rd_eng[k] = {}
                rd_dma[k] = []
            for k in op.r:
                if k in op.w:
                    continue
                if op.dma:
                    rd_dma.setdefault(k, []).append(i)
                else:
                    rd_eng.setdefault(k, {})[op.eng] = i
        engs = ['pe', 'act', 'dve', 'pool', 'sp']
        if Stage.SEMS is None or Stage.SEMS[0] is not nc:
            es0 = ExitStack()
            prog0 = {e: es0.enter_context(nc.semaphore(f"pg_{e}")) for e in ['pe', 'act', 'dve', 'pool']}
            dsem0 = {q: [es0.enter_context(nc.semaphore(f"d_{q}{i}")) for i in range(n)]
                     for q, n in self.NSEM.items()}
            Stage.SEMS = (nc, prog0, dsem0, es0)
            Stage.DVAL = {}
            dval = Stage.DVAL
        with ExitStack() as es:
            prog, dsem = Stage.SEMS[1], Stage.SEMS[2]
            cnt = {e: 0 for e in prog}
            duse = {q: 0 for q in dsem}
            dval = Stage.DVAL
            for op in ops:
                if op.dma:
                    q = 'cc' if op.inc != 16 else op.eng
                    lst = dsem[q]
                    sm = lst[duse[q] % len(lst)]
                    duse[q] += 1
                    op.sem = sm
                    op.prev = dval.get(sm.name if hasattr(sm, 'name') else id(sm), 0)
                    op.val = op.prev + op.inc
                    dval[sm.name if hasattr(sm, 'name') else id(sm)] = op.val
                elif op.need:
                    cnt[op.eng] += 1
                    op.ms = cnt[op.eng]
            per = {e: [] for e in engs}
            for op in ops:
                per[op.eng].append(op)

            def emit(eng_name, e):
                waited = {}

                def wait(sm, val):
                    key = id(sm)
                    if waited.get(key, 0) >= val:
                        return
                    waited[key] = val
                    e.wait_ge(sm, val)

                for op in per[eng_name]:
                    for j in op.deps:
                        pj = ops[j]
                        if pj.dma:
                            wait(pj.sem, pj.val)
                        else:
                            wait(prog[pj.eng], pj.ms)
                    if op.dma and op.prev > 0:
                        wait(op.sem, op.prev)
                    ins = op.fn(e)
                    if op.dma:
                        ins.then_inc(op.sem, op.inc)
                    elif op.need:
                        ins.then_inc(prog[op.eng], 1)
                if eng_name in dsem or eng_name == 'pool':
                    fin = {}
                    for op in per[eng_name]:
                        if op.dma:
                            fin[id(op.sem)] = (op.sem, op.val)
                    for sm, val in fin.values():
                        wait(sm, val)

            with nc.Block() as blk:
                @blk.tensor
                def _(e):
                    emit('pe', e)

                @blk.scalar
                def _(e):
                    emit('act', e)

                @blk.vector
                def _(e):
                    emit('dve', e)

                @blk.gpsimd
                def _(e):
                    emit('pool', e)

                @blk.sync
                def _(e):
                    emit('sp', e)
            with nc.Block() as blk2:
                @blk2.gpsimd
                def _(e):
                    for sm in list(prog.values()) + dsem['sp'] + dsem['act']:
                        e.sem_clear(sm)
                    for sm in dsem['sp'] + dsem['act']:
                        Stage.DVAL.pop(sm.name if hasattr(sm, 'name') else id(sm), None)
        self.es.close()
        self.ops = []


class Ring:
    def __init__(self, items):
        self.items = items
        self.i = 0

    def next(self):
        it = self.items[self.i % len(self.items)]
        self.i += 1
        return it


class K:
    pass


def input_specs(c):
    D, KC, H, NCORE = c['D'], c['KC'], c['HPC'], c['NCORE']
    sp = {
        'x_own': (c['TL'], D), 'ctx_own': (c['TC'], D), 'cT': (128, KC * 3), 'sel': (3, 2), 'ident': (128, 128),
        'oh': (128, c['CPB']), 'ohb': (128, c['B']), 'wr': (128, KC * c['NE']), 'rb': (1, c['NE']), 'fnorm': (1, D),
        'c64': (64, c['TB']), 's64': (64, c['TB']), 'c128': (128, c['TB']), 's128': (128, c['TB']),
    }
    for l in range(c['DEPTH']):
        sp.update({
            f'wmod{l}': (D, c['MS']), f'bmod{l}': (1, c['MS']), f'wpc{l}': (D, c['NCOL']),
            f'wbg{l}': (4 * D // NCORE, D), f'wb{l}': (4 * c['BW'] // NCORE, D), f'wout{l}': (D // NCORE, D),
            f'weg{l}': (c['NE'] * D // NCORE, c['DE']), f'weu{l}': (c['NE'] * D // NCORE, c['DE']),
            f'wed{l}': (c['NE'] * c['DE'] // NCORE, D),
            f'nmix{l}': (128, KC), f'nffn{l}': (128, KC), f'qn{l}': (128, c['QL'] // 128), f'kvn{l}': (128, c['KVL'] // 128),
            f'wuq{l}': (c['QL'], H * 256), f'wukv{l}': (c['KVL'], H * 256),
            f'gqn{l}': (128, 2), f'gkn{l}': (128, 2), f'nab{l}': (H * c['WH'] * 128, 256),
            f'lcw{l}': (128, H * 4), f'lcb{l}': (128, H), f'lwa{l}': (128, 2 * H * 128), f'lwx{l}': (128, 2 * H * 128),
            f'lba{l}': (128, 2 * H), f'lbx{l}': (128, 2 * H), f'llam{l}': (128, 2 * H), f'bbg{l}': (128, 4 * KC),
        })
    return sp


def build(c, only=None):
    nc = bass.Bass("TRN2", target_bir_lowering=False)
    k = K()
    D, KC, H, NCORE, CPB = c['D'], c['KC'], c['HPC'], c['NCORE'], c['CPB']
    TL, TC, TOK, TB, CTX, S = c['TL'], c['TC'], c['TOK'], c['TB'], c['CTX'], c['S']
    QL, KVL, DE, NE, BW, NH = c['QL'], c['KVL'], c['DE'], c['NE'], c['BW'], c['NH']
    MS, NCOL, SEG = c['MS'], c['NCOL'], c['SEG']
    specs_ = input_specs(c)
    I = {n: nc.dram_tensor(n, list(specs_[n]), F32, kind="ExternalInput").ap() for n in specs_ if (only is None or n in only)}
    k.I = I
    out_own = nc.dram_tensor("out_own", [TL, D], F32, kind="ExternalOutput").ap()

    def scr(name, shape, dt=BF16):
        return nc.dram_tensor(name, list(shape), dt, kind="Internal").ap()

    ALLG = [list(range(NCORE))]
    BG = [list(range(b * CPB, (b + 1) * CPB)) for b in range(c['B'])]
    xres = scr("xres", [TOK, D], F32)
    xmid = scr("xmid", [TOK, D], F32)

    def own_tiles(with_ctx=True):
        tl = []
        if with_ctx:
            tl.append(dict(ctx=True, col0=0, n=TC, blocks=[(r, min(128, TC - r)) for r in range(0, TC, 128)]))
        for i in range(TL // 512):
            tl.append(dict(ctx=False, col0=TC + 512 * i, n=512, blocks=[(TC + 512 * i + 128 * b, 128) for b in range(4)]))
        return tl

    def batch_tiles(with_ctx=True):
        tl = []
        if with_ctx:
            for t0 in range(0, CTX, 512):
                tl.append(dict(ctx=True, t0=t0, n=min(512, CTX - t0)))
        for i in range(S // 512):
            tl.append(dict(ctx=False, t0=CTX + 512 * i, n=512))
        return tl

    def gathered_pieces(t0, n):
        res = []
        t = t0
        while t < t0 + n:
            if t < CTX:
                r = t // TC
                o = t % TC
                m = min(TC - o, t0 + n - t)
                res.append((r, o, m, t - t0))
            else:
                u = t - CTX
                r = u // TL
                o = u % TL
                m = min(TL - o, t0 + n - t)
                res.append((r, TC + o, m, t - t0))
            t += m
        return res

    wfull = {}

    WL = {'wbg': dict(s=1, cb=128), 'wb': dict(s=1, cb=128), 'wout': dict(s=1, cb=512),
          'weg': dict(s=NE // NCORE, cb=128), 'weu': dict(s=NE // NCORE, cb=128), 'wed': dict(s=NE // NCORE, cb=512)}

    def wview(nm, l):
        full = wfull[f'{nm}{l}']
        R, C = I[f'{nm}{l}'].shape
        S_ = WL[nm]['s']; cb = WL[nm]['cb']
        K_ = R // (S_ * 128)
        return full.rearrange("a b -> (a b)").rearrange("(r s f p k c) -> r s f p k c", r=NCORE, s=S_, f=C // cb, p=128, k=K_, c=cb), K_

    def w_ops(st, l):
        for nm in ['wbg', 'wb', 'wout', 'weg', 'weu', 'wed']:
            src = I[f'{nm}{l}']
            R, C = src.shape
            sh = scr(f"{nm}{l}_bs", [R, C])
            full = scr(f"{nm}{l}_bf", [R * NCORE, C])
            S_ = WL[nm]['s']; cb = WL[nm]['cb']
            K_ = R // (S_ * 128)
            F_ = C // cb
            shv = sh.rearrange("a b -> (a b)").rearrange("(s f p k c) -> s f p k c", s=S_, f=F_, p=128, k=K_, c=cb)
            for si in range(S_):
                for f in range(F_):
                    st.dma(shv[si, f], src[si * K_ * 128:(si + 1) * K_ * 128, f * cb:(f + 1) * cb].rearrange("(k p) c -> p k c", p=128),
                           r=[], w=[f'{nm}sh'], q='pool')
            st.cc("AllGather", ALLG, sh, full, r=[f'{nm}sh'], w=[f'{nm}full'])
            wfull[f'{nm}{l}'] = full

    def stage_w(l):
        st = Stage(nc, f"W{l}")
        src = I[f'wpc{l}']
        dst = scr(f"wpc{l}_bf", [D, NCOL])
        pieces = max(1, -(-NCOL * 4 // 4096))
        step = max(1, 4096 // pieces)
        for r0 in range(0, D, step):
            r1 = min(D, r0 + step)
            st.dma(dst[r0:r1, :], src[r0:r1, :], r=[], w=['wpc'], q='pool', max_dma_last_dim=4096)
        wfull[f'wpc{l}'] = dst
        st.run()

    modown = {}
    modT = {}

    def stage_m(l):
        rows_sh = scr(f"modrow_sh{l}", [3, MS], F32)
        rows_all = scr(f"modrow_all{l}", [3 * NCORE, MS], F32)
        modown[l] = scr(f"modown{l}", [2, 6 * D], F32)
        modT[l] = scr(f"modT{l}", [128, 6 * KC * 2], F32)
        st = Stage(nc, f"M{l}")
        sT = st.sb([128, KC * 3], F32, 'sT')
        st.dma(sT[:], I['cT'], r=[], w=['sT'])
        st.act(sT[:], sT[:], AF.Silu, r=['sT'], w=['sT'])
        bm = st.sb([3, MS], F32, 'bm')
        st.dma(bm[:], I[f'bmod{l}'].partition_broadcast(3), r=[], w=['bm'])
        mrow = st.sb([3, MS], F32, 'mrow')
        CW = 256
        ring = Ring([(st.sb([128, KC, CW], F32, 'wm'), f'wm{i}') for i in range(2)])
        pr = Ring([(st.ps(), f'ps{i}') for i in range(2)])
        wv = I[f'wmod{l}'].rearrange("(k p) c -> p k c", p=128)
        for c0 in range(0, MS, CW):
            cw = min(CW, MS - c0)
            wt, wk = ring.next()
            st.dma(wt[:, :, :cw], wv[:, :, c0:c0 + cw], r=[], w=[wk])
            ps, pk = pr.next()
            for kk in range(KC):
                st.mm(ps[0:3, :cw], sT[:, kk * 3:kk * 3 + 3], wt[:, kk, :cw], kk == 0, kk == KC - 1, r=['sT', wk], w=[pk])
            st.dve(lambda e, ps=ps, c0=c0, cw=cw: e.tensor_tensor(mrow[:, c0:c0 + cw], ps[0:3, :cw], bm[:, c0:c0 + cw], ALU.add),
                   r=[pk, 'bm'], w=['mrow'])
        st.dma(rows_sh, mrow[:], r=['mrow'], w=['rows_sh'])
        st.cc("AllGather", ALLG, rows_sh, rows_all, r=['rows_sh'], w=['rows_all'])
        st.run()
        st = Stage(nc, f"N{l}")
        selt = st.sb([3, 2], F32, 'sel')
        st.dma(selt[:], I['sel'], r=[], w=['sel'])
        idn = st.sb([128, 128], F32, 'idn')
        st.dma(idn[:], I['ident'], r=[], w=['idn'])
        m3r = Ring([(st.sb([3, MS], F32, 'm3'), f'm3{i}') for i in range(2)])
        mor = Ring([(st.sb([2, MS], F32, 'mo'), f'mo{i}') for i in range(2)])
        pr = Ring([(st.ps(), f'ps{i}') for i in range(2)])
        pt = st.ps('pt')
        nb = 6 * KC
        nbr = MS // 128
        for r in range(NCORE):
            m3, m3k = m3r.next()
            st.dma(m3[:], rows_all[r * 3:(r + 1) * 3, :], r=[], w=[m3k])
            mo, mok = mor.next()
            for c0 in range(0, MS, 512):
                cw = min(512, MS - c0)
                ps, pk = pr.next()
                st.mm(ps[0:2, :cw], selt[:, :], m3[:, c0:c0 + cw], True, True, r=['sel', m3k], w=[pk])
                st.dve(lambda e, ps=ps, mo=mo, c0=c0, cw=cw: e.tensor_copy(mo[:, c0:c0 + cw], ps[0:2, :cw]), r=[pk], w=[mok])
            st.dma(modown[l][:, r * MS:(r + 1) * MS], mo[:], r=[mok], w=['modown'], q='pool')
            for blk in range(nbr):
                gb_ = r * nbr + blk
                st.tr(pt[:, gb_ * 2:gb_ * 2 + 2], mo[:, blk * 128:(blk + 1) * 128], idn[0:2, 0:2], r=[mok, 'idn'], w=['pt'])
        mt = st.sb([128, nb * 2], F32, 'mt')
        st.dve(lambda e: e.tensor_copy(mt[:], pt[:, :nb * 2]), r=['pt'], w=['mt'])
        st.dma(modT[l], mt[:], r=['mt'], w=['modT'])
        st.run()

    def load_mod(st, l, gain_name, m_shift, m_scale):
        mt = st.sb([128, 6 * KC, 2], F32, 'mt')
        st.dma(mt[:], modT[l].rearrange("p (a i) -> p a i", i=2), r=[], w=['mt'])
        gn = st.sb([128, KC], F32, 'gn')
        st.dma(gn[:], I[f'{gain_name}{l}'], r=[], w=['gn'])
        A = st.sb([128, KC, 2], F32, 'A')
        for i in range(2):
            st.dve(lambda e, i=i: e.scalar_tensor_tensor(A[:, :, i], mt[:, m_scale * KC:(m_scale + 1) * KC, i], 1.0, gn[:],
                                                         ALU.add, ALU.mult), r=['mt', 'gn'], w=['A'])
        return A, mt

    def xsrc_rows(l, r0, nt, which):
        if which == 'in':
            if r0 < TC:
                return I['ctx_own'][r0:r0 + nt, :]
            return I['x_own'][r0 - TC:r0 - TC + nt, :]
        return (xres if which == 'res' else xmid)[r0:r0 + nt, :]

    def norm_tiles(st, l, which, A, mt, m_shift, tiles, consume, want_f32=False):
        idn = st.sb([128, 128], F32, 'idn')
        st.dma(idn[:], I['ident'], r=[], w=['idn'])
        xr = Ring([(st.sb([128, D], F32, 'x'), f'x{i}') for i in range(2)])
        junk = st.sb([128, D], BF16, 'junk')
        ssr = Ring([(st.sb([128, 4], F32, 'ss'), f'ss{i}') for i in range(2)])
        pr = Ring([(st.ps(), f'pt{i}') for i in range(3)])
        hr = Ring([(st.sb([128, KC, 512], BF16, 'hT'), f'hT{i}') for i in range(2)])
        for tile in tiles:
            hT, hk = hr.next()
            i = 1 if tile['ctx'] else 0
            for bi, (r0, nt) in enumerate(tile['blocks']):
                xt, xk = xr.next()
                ss, sk = ssr.next()
                st.dma(xt[:nt, :], xsrc_rows(l, r0, nt, which), r=[], w=[xk])
                st.act(junk[:nt, :], xt[:nt, :], AF.Square, r=[xk], w=['junk', sk])
                st.ops[-1].fn = (lambda e, xt=xt, ss=ss, nt=nt: e.activation(junk[:nt, :], xt[:nt, :], AF.Square, accum_out=ss[:nt, 0:1]))
                st.dve(lambda e, ss=ss, nt=nt: e.tensor_scalar(ss[:nt, 1:2], ss[:nt, 0:1], 1.0 / D, 1e-6, ALU.mult, ALU.add), r=[sk], w=[sk])
                st.act(ss[:nt, 3:4], ss[:nt, 1:2], AF.Sqrt, r=[sk], w=[sk])
                st.dve(lambda e, ss=ss, nt=nt: e.reciprocal(ss[:nt, 2:3], ss[:nt, 3:4]), r=[sk], w=[sk])
                if DBG_LEVEL < 2:
                    continue
                st.act(xt[:nt, :], xt[:nt, :], AF.Copy, r=[xk, sk], w=[xk], scale=ss[:nt, 2:3])
                c0 = r0 - tile['blocks'][0][0]
                for kg in range(0, KC if DBG_LEVEL >= 3 else 0, 4):
                    ps, pk = pr.next()
                    for q in range(4):
                        st.tr(ps[:, q * 128:q * 128 + nt], xt[:nt, (kg + q) * 128:(kg + q + 1) * 128], idn[:nt, :nt], r=[xk, 'idn'], w=[pk])
                    for q in range(4):
                        kk = kg + q
                        st.act(hT[:, kk, c0:c0 + nt], ps[:, q * 128:q * 128 + nt], AF.Identity, r=[pk, 'A', 'mt'], w=[hk],
                               bias=mt[:, m_shift * KC + kk, i:i + 1], scale=A[:, kk, i:i + 1])
                        if want_f32:
                            want_f32(st, tile, bi, kk, ps[:, q * 128:q * 128 + nt], pk, A[:, kk, i:i + 1], mt[:, m_shift * KC + kk, i:i + 1], nt)
            if DBG_LEVEL >= 4:
                consume(st, tile, hT, hk)

    hT_sh = scr("hT_sh", [D, TOK])
    hT_all = scr("hT_all", [CPB * D, TOK])
    hT_g = scr("hT_g", [NCORE * D, TOK])

    def gather_select(name, sh, big, out, rows, cols, kk):
        st = Stage(nc, name)
        stg = scr(name + "_stg", list(sh.shape))
        nr = sh.shape[0]
        step = max(1, nr // 4)
        for r0 in range(0, nr, step):
            st.dma(stg[r0:min(nr, r0 + step), :], sh[r0:min(nr, r0 + step), :], r=[], w=['stg'], q='pool')
        st.cc("AllGather", ALLG, stg, big, r=['stg'], w=['big'])
        ohb = st.sb([128, c['B']], F32, 'ohb')
        st.dma(ohb[:], I['ohb'], r=[], w=['ohb'])
        rings = [Ring([(st.sb([128, kk, cols], BF16, f'c{b}'), f'c{b}_{i}') for i in range(2)]) for b in range(c['B'])]
        bv = big.rearrange("(r k p) t -> r p k t", p=128, k=rows // 128)
        ov = out.rearrange("(r k p) t -> r p k t", p=128, k=rows // 128)
        for j in range(CPB if DBG_LEVEL >= 6 else 0):
            for k0 in range(0, rows // 128, kk):
                k1 = min(rows // 128, k0 + kk)
                tl = []
                for b in range(c['B']):
                    t, tk = rings[b].next()
                    st.dma(t[:, :k1 - k0, :], bv[b * CPB + j, :, k0:k1, :], r=['big'], w=[tk])
                    tl.append((t, tk))
                t0, tk0 = tl[0]
                if DBG_LEVEL < 7:
                    continue
                st.dve(lambda e, t0=t0, n_=k1 - k0: e.tensor_scalar(t0[:, :n_, :], t0[:, :n_, :], ohb[:, 0:1], None, ALU.mult), r=[tk0, 'ohb'], w=[tk0])
                for b in range(1, c['B']):
                    tb, tkb = tl[b]
                    st.dve(lambda e, t0=t0, tb=tb, b=b, n_=k1 - k0: e.scalar_tensor_tensor(t0[:, :n_, :], tb[:, :n_, :], ohb[:, b:b + 1], t0[:, :n_, :], ALU.mult, ALU.add), r=[tk0, tkb, 'ohb'], w=[tk0])
                if DBG_LEVEL >= 8:
                    st.dma(ov[j, :, k0:k1, :], t0[:, :k1 - k0, :], r=[tk0], w=['out'], q='pool')
        st.run()


    def stage_1(l):
        st = Stage(nc, f"S1_{l}")
        A, mt = load_mod(st, l, 'nmix', 0, 1)
        hv = hT_sh.rearrange("(k p) t -> p k t", p=128)

        def consume(st, tile, hT, hk):
            st.dma(hv[:, :, tile['col0']:tile['col0'] + tile['n']], hT[:, :, :tile['n']], r=[hk], w=['hT_sh'], q='pool')

        norm_tiles(st, l, 'in' if l == 0 else 'res', A, mt, 0, own_tiles(True), consume)
        st.run()
        if DBG_LEVEL >= 5:
            gather_select(f"S1G_{l}", hT_sh, hT_g, hT_all, D, TOK, 4)

    projT = scr("projT", [NCOL, TB])
    gvtok = scr("gvtok", [TB, 128])
    nvtok = scr("nvtok", [TB, H * 128])
    yT_sh = scr("yT_sh", [4 * H * 128, TB])
    yT_all = scr("yT_all", [CPB * 4 * H * 128, TB])

    def load_hT_batch(st, dst, dk, t0, n):
        hv = hT_all.rearrange("(r k p) t -> r p k t", p=128, k=KC)
        for (r, o, m, d0) in gathered_pieces(t0, n):
            st.dma(dst[:, :, d0:d0 + m], hv[r, :, :, o:o + m], r=['hT_all'], w=[dk])

    def stage_2(l):
        st = Stage(nc, f"S2_{l}")
        w_ops(st, l)
        wv = wfull[f'wpc{l}'].rearrange("(k p) c -> p k c", p=128)
        wr_ = Ring([(st.sb([128, KC, 512], BF16, 'w'), f'w{i}') for i in range(2)])
        hr = Ring([(st.sb([128, KC, 512], BF16, 'h'), f'h{i}') for i in range(2)])
        pr = Ring([(st.ps(), f'ps{i}') for i in range(4)])
        orr = Ring([(st.sb([128, 4, 512], BF16, 'o'), f'o{i}') for i in range(2)])
        FM = SEG['gv'][0]
        pieces = [('fm', p0, min(512, FM - p0)) for p0 in range(0, FM, 512)] + [('tm', FM, NCOL - FM)]
        for (kind, c0, w) in pieces:
            wt, wk = wr_.next()
            st.dma(wt[:, :, :w], wv[:, :, c0:c0 + w], r=['wpc'], w=[wk])
            for tile in batch_tiles(True):
                t0, n = tile['t0'], tile['n']
                ht, hk = hr.next()
                load_hT_batch(st, ht, hk, t0, n)
                ot, ok_ = orr.next()
                if kind == 'fm':
                    ncc = -(-w // 128)
                    for cc in range(ncc):
                        cw = min(128, w - cc * 128)
                        ps, pk = pr.next()
                        for kk in range(KC):
                            st.mm(ps[:cw, :n], wt[:, kk, cc * 128:cc * 128 + cw], ht[:, kk, :n], kk == 0, kk == KC - 1, r=[wk, hk], w=[pk])
                        st.act(ot[:cw, cc, :n], ps[:cw, :n], AF.Copy, r=[pk], w=[ok_])
                    assert w % 128 == 0
                    st.dma(projT[c0:c0 + w, t0:t0 + n].rearrange("(cc p) t -> p cc t", p=128), ot[:, :ncc, :n], r=[ok_], w=['projT'], q='act')
                else:
                    ntb = n // 128
                    for (dstt, cb, cw) in ((gvtok, 0, 128), (nvtok, 128, H * 128)):
                        ot, ok_ = orr.next()
                        for tb in range(ntb):
                            ps, pk = pr.next()
                            for kk in range(KC):
                                st.mm(ps[:, :cw], ht[:, kk, tb * 128:(tb + 1) * 128], wt[:, kk, cb:cb + cw], kk == 0, kk == KC - 1, r=[wk, hk], w=[pk])
                            st.dve(lambda e, ps=ps, ot=ot, tb=tb, cw=cw: e.tensor_copy(ot[:, tb, :cw], ps[:, :cw]), r=[pk], w=[ok_])
                        st.dma(dstt[t0:t0 + n, 0:cw].rearrange("(tb p) c -> p tb c", p=128), ot[:, :ntb, :cw], r=[ok_], w=['vtok'], q='act')
        st.run()

    def rstd_bc(st, out_ap, ps_ap, nfeat, tmp_ap, keys_r, keys_w):
        st.dve(lambda e: e.tensor_scalar(tmp_ap, ps_ap, 1.0 / nfeat, 1e-6, ALU.mult, ALU.add), r=keys_r, w=keys_w)
        st.act(tmp_ap, tmp_ap, AF.Sqrt, r=keys_w, w=keys_w)
        st.dve(lambda e: e.reciprocal(out_ap, tmp_ap), r=keys_w, w=keys_w)

    class AttnCtx:
        def __init__(self, st):
            self.st = st
            self.ones = st.sb([128, 128], BF16, 'ones')
            st.pool(lambda e: e.memset(self.ones[:], 1.0), r=[], w=['ones'])
            self.sp = Ring([(st.ps(), f'aS{i}') for i in range(2)])
            self.op = Ring([(st.ps(), f'aO{i}') for i in range(2)])
            self.mp = Ring([(st.ps(), f'aM{i}') for i in range(2)])
            self.pt = Ring([(st.sb([128, 512], BF16, 'PT'), f'PT{i}') for i in range(3)])
            self.rs = Ring([(st.sb([128, 512], F32, 'rs'), f'rs{i}') for i in range(2)])
            self.y = Ring([(st.sb([128, 512], BF16, 'y'), f'y{i}') for i in range(2)])

    def attention(ac, qparts, kparts, vfn, kcs, q0, n, scale, yrow0, rk):
        st = ac.st
        O, ok_ = ac.op.next()
        M, mk = ac.mp.next()
        def pv(ci, kc, PT, pk):
            va, vk = vfn(kc)
            st.mm(O[:, :n], va, PT[:, :n], ci == 0, ci == len(kcs) - 1, r=[pk, vk] + rk, w=[ok_])
            st.mm(M[:, :n], ac.ones[:], PT[:, :n], ci == 0, ci == len(kcs) - 1, r=[pk, 'ones'], w=[mk])

        pend = None
        for ci, kc in enumerate(kcs):
            Sp, sk = ac.sp.next()
            for i in range(len(qparts)):
                st.mm(Sp[:, :n], kparts[i][0](kc), qparts[i][0](q0, n), i == 0, i == len(qparts) - 1,
                      r=[qparts[i][1], kparts[i][1]] + rk, w=[sk])
            PT, pk = ac.pt.next()
            st.act(PT[:, :n], Sp[:, :n], AF.Exp, r=[sk], w=[pk], scale=scale)
            if pend is not None:
                pv(*pend)
            pend = (ci, kc, PT, pk)
        pv(*pend)
        rs, rkx = ac.rs.next()
        st.dve(lambda e: e.reciprocal(rs[:, :n], M[:, :n]), r=[mk], w=[rkx])
        y, yk = ac.y.next()
        st.dve(lambda e: e.tensor_tensor(y[:, :n], O[:, :n], rs[:, :n], ALU.mult), r=[ok_, rkx], w=[yk])
        st.dma(yT_sh[yrow0:yrow0 + 128, q0:q0 + n], y[:, :n], r=[yk], w=['yT_sh'], q='pool')

    NKC = TB // 128
    CKC = CTX // 128

    def run_attention(ac, qparts, kparts, vfn, scale, yrow0, need_ctx, rk):
        if need_ctx:
            for t0 in range(0, CTX, 512):
                attention(ac, qparts, kparts, vfn, list(range(CKC)), t0, min(512, CTX - t0), scale, yrow0, rk)
        for i in range(S // 512):
            attention(ac, qparts, kparts, vfn, list(range(NKC)), CTX + 512 * i, 512, scale, yrow0, rk)

    def stage_3b(l, need_ctx):
        st = Stage(nc, f"S3B_{l}")
        ac = AttnCtx(st)
        gq = st.sb([128, 2], F32, 'gq'); gk = st.sb([128, 2], F32, 'gk')
        st.dma(gq[:], I[f'gqn{l}'], r=[], w=['gq']); st.dma(gk[:], I[f'gkn{l}'], r=[], w=['gk'])
        KT = st.sb([128, TB], BF16, 'KT')
        QT = st.sb([128, TB], BF16, 'QT')
        V = st.sb([128, NKC, 128], BF16, 'V')
        gvv = gvtok.rearrange("(c p) d -> p c d", p=128)
        for c0_ in range(0, NKC, 16):
            c1_ = min(NKC, c0_ + 16)
            st.dma(V[:, c0_:c1_, :], gvv[:, c0_:c1_, :], r=['vtok'], w=['V'])
        xr = Ring([(st.sb([128, 2, 512], BF16, 'xr'), f'xr{i}') for i in range(2)])
        tb_ = Ring([(st.sb([128, 2, 512], F32, 'tb'), f'tb{i}') for i in range(2)])
        wk_ = Ring([(st.sb([128, 4, 512], F32, 'wk'), f'wk{i}') for i in range(2)])
        sq_ = Ring([(st.sb([128, 512], BF16, 'sq'), f'sq{i}') for i in range(2)])
        pp = Ring([(st.ps(), f'pp{i}') for i in range(2)])

        def prep(seg, segs, gn, gkey, dst, dkey):
            o1 = SEG[seg][0]; o2 = SEG[segs][0]
            for tile in batch_tiles(True):
                t0, n = tile['t0'], tile['n']
                x, xk = xr.next(); tb, tk = tb_.next(); wk, wkk = wk_.next(); sq, sqk = sq_.next(); ps, pk = pp.next()
                st.dma(x[:, 0, :n], projT[o1:o1 + 128, t0:t0 + n], r=['projT'], w=[xk])
                st.dma(x[:, 1, :n], projT[o2:o2 + 128, t0:t0 + n], r=['projT'], w=[xk])
                st.dma(tb[:, 0, :n], I['c128'][:, t0:t0 + n], r=[], w=[tk])
                st.dma(tb[:, 1, :n], I['s128'][:, t0:t0 + n], r=[], w=[tk])
                st.dve(lambda e, x=x, sq=sq, n=n: e.tensor_tensor(sq[:, :n], x[:, 0, :n], x[:, 0, :n], ALU.mult), r=[xk], w=[sqk])
                st.mm(ps[:, :n], ac.ones[:], sq[:, :n], True, True, r=[sqk, 'ones'], w=[pk])
                rstd_bc(st, wk[:, 0, :n], ps[:, :n], 128.0, wk[:, 1, :n], [pk], [wkk])
                st.dve(lambda e, x=x, wk=wk, n=n: e.scalar_tensor_tensor(wk[:, 1, :n], x[:, 0, :n], gn[:, 0:1], wk[:, 0, :n], ALU.mult, ALU.mult), r=[xk, wkk, gkey], w=[wkk])
                st.dve(lambda e, x=x, wk=wk, n=n: e.scalar_tensor_tensor(wk[:, 2, :n], x[:, 1, :n], gn[:, 1:2], wk[:, 0, :n], ALU.mult, ALU.mult), r=[xk, wkk, gkey], w=[wkk])
                st.dve(lambda e, wk=wk, tb=tb, n=n: e.tensor_tensor(wk[:, 1, :n], wk[:, 1, :n], tb[:, 0, :n], ALU.mult), r=[wkk, tk], w=[wkk])
                st.dve(lambda e, wk=wk, tb=tb, n=n: e.tensor_tensor(wk[:, 2, :n], wk[:, 2, :n], tb[:, 1, :n], ALU.mult), r=[wkk, tk], w=[wkk])
                st.dve(lambda e, wk=wk, n=n, t0=t0: e.tensor_tensor(dst[:, t0:t0 + n], wk[:, 1, :n], wk[:, 2, :n], ALU.add), r=[wkk], w=[dkey])

        prep('gk', 'gks', gk, 'gk', KT, 'KT')
        for hh in range(H):
            SEG_q = SEG['gq'][0] + hh * 128; SEG_qs = SEG['gqs'][0] + hh * 128
            SEG['_q'] = (SEG_q, 128); SEG['_qs'] = (SEG_qs, 128)
            prep('_q', '_qs', gq, 'gq', QT, 'QT')
            run_attention(ac, [(lambda q0, n: QT[:, q0:q0 + n], 'QT')], [(lambda kc: KT[:, kc * 128:(kc + 1) * 128], 'KT')],
                          lambda kc: (V[:, kc, :], 'V'), 128.0 ** -0.5, (1 * H + hh) * 128, need_ctx, [])
        SEG.pop('_q'); SEG.pop('_qs')
        st.run()

    def stage_3a(l, need_ctx):
        QLc, KVc = QL // 128, KVL // 128
        for hh in range(H):
            st = Stage(nc, f"S3A_{l}_{hh}")
            ac = AttnCtx(st)
            qn = st.sb([128, QLc], F32, 'qn'); kvn = st.sb([128, KVc], F32, 'kvn')
            st.dma(qn[:], I[f'qn{l}'], r=[], w=['qn']); st.dma(kvn[:], I[f'kvn{l}'], r=[], w=['kvn'])
            wuq = st.sb([128, QLc, 256], BF16, 'wuq'); wukv = st.sb([128, KVc, 256], BF16, 'wukv')
            st.dma(wuq[:], I[f'wuq{l}'][:, hh * 256:(hh + 1) * 256].rearrange("(k p) c -> p k c", p=128), r=[], w=['wuq'], q='pool')
            st.dma(wukv[:], I[f'wukv{l}'][:, hh * 256:(hh + 1) * 256].rearrange("(k p) c -> p k c", p=128), r=[], w=['wukv'], q='pool')
            KR = st.sb([64, TB], BF16, 'KR'); QN = st.sb([128, TB], BF16, 'QN'); QR = st.sb([64, TB], BF16, 'QR')
            KN = st.sb([128, TB], BF16, 'KN'); V = st.sb([128, NKC, 128], BF16, 'V')
            cqr = Ring([(st.sb([128, QLc + KVc, 512], BF16, 'cq'), f'cq{i}') for i in range(2)])
            sq = st.sb([128, QLc + KVc, 512], BF16, 'sq')
            cn = st.sb([128, QLc + KVc, 512], BF16, 'cn')
            krr = Ring([(st.sb([64, 2, 512], BF16, 'kr'), f'kr{i}') for i in range(2)])
            tbr = Ring([(st.sb([64, 2, 512], F32, 'tb'), f'tb{i}') for i in range(2)])
            wk = st.sb([128, 4, 512], F32, 'wk')
            pp = Ring([(st.ps(), f'pp{i}') for i in range(2)])
            ocq, ockv, okr, okrs = SEG['cq'][0], SEG['ckv'][0], SEG['kr'][0], SEG['krs'][0]
            for tile in batch_tiles(True):
                t0, n = tile['t0'], tile['n']
                cq, ck = cqr.next(); kr, kk_ = krr.next(); tb, tk = tbr.next()
                st.dma(cq[:, :QLc, :n], projT[ocq:ocq + QL, t0:t0 + n].rearrange("(k p) t -> p k t", p=128), r=['projT'], w=[ck])
                st.dma(cq[:, QLc:, :n], projT[ockv:ockv + KVL, t0:t0 + n].rearrange("(k p) t -> p k t", p=128), r=['projT'], w=[ck])
                st.dma(kr[:, 0, :n], projT[okr:okr + 64, t0:t0 + n], r=['projT'], w=[kk_])
                st.dma(kr[:, 1, :n], projT[okrs:okrs + 64, t0:t0 + n], r=['projT'], w=[kk_])
                st.dma(tb[:, 0, :n], I['c64'][:, t0:t0 + n], r=[], w=[tk])
                st.dma(tb[:, 1, :n], I['s64'][:, t0:t0 + n], r=[], w=[tk])
                st.dve(lambda e, kr=kr, tb=tb, n=n: e.tensor_tensor(wk[0:64, 0, :n], kr[:, 0, :n], tb[:, 0, :n], ALU.mult), r=[kk_, tk], w=['wk'])
                st.dve(lambda e, kr=kr, tb=tb, n=n: e.tensor_tensor(wk[0:64, 1, :n], kr[:, 1, :n], tb[:, 1, :n], ALU.mult), r=[kk_, tk], w=['wk'])
                st.dve(lambda e, n=n, t0=t0: e.tensor_tensor(KR[:, t0:t0 + n], wk[0:64, 0, :n], wk[0:64, 1, :n], ALU.add), r=['wk'], w=['KR'])
                st.dve(lambda e, cq=cq, n=n: e.tensor_tensor(sq[:, :, :n], cq[:, :, :n], cq[:, :, :n], ALU.mult), r=[ck], w=['sq'])
                for (c0, cc, gn, gkey, nf, slot) in ((0, QLc, qn, 'qn', QL, 2), (QLc, KVc, kvn, 'kvn', KVL, 3)):
                    ps, pk = pp.next()
                    for kk in range(cc):
                        st.mm(ps[:, :n], ac.ones[:], sq[:, c0 + kk, :n], kk == 0, kk == cc - 1, r=['sq', 'ones'], w=[pk])
                    rstd_bc(st, wk[:, slot, :n], ps[:, :n], float(nf), wk[:, 0, :n], [pk, 'wk'], ['wk'])
                    for kk in range(cc):
                        st.dve(lambda e, cq=cq, c0=c0, kk=kk, gn=gn, slot=slot, n=n: e.scalar_tensor_tensor(
                            cn[:, c0 + kk, :n], cq[:, c0 + kk, :n], gn[:, kk:kk + 1], wk[:, slot, :n], ALU.mult, ALU.mult), r=[ck, 'wk', gkey], w=['cn'])
                for (wt, wkey, c0, cc, col, dst, dkey) in ((wuq, 'wuq', 0, QLc, 0, QN, 'QN'), (wukv, 'wukv', QLc, KVc, 0, KN, 'KN')):
                    ps, pk = pp.next()
                    for kk in range(cc):
                        st.mm(ps[:, :n], wt[:, kk, col:col + 128], cn[:, c0 + kk, :n], kk == 0, kk == cc - 1, r=['cn', wkey], w=[pk])
                    st.act(dst[:, t0:t0 + n], ps[:, :n], AF.Copy, r=[pk], w=[dkey])
                ps1, pk1 = pp.next()
                for kk in range(QLc):
                    st.mm(ps1[0:64, :n], wuq[:, kk, 128:192], cn[:, kk, :n], kk == 0, kk == QLc - 1, r=['cn', 'wuq'], w=[pk1])
                st.dve(lambda e, ps1=ps1, tb=tb, n=n: e.tensor_tensor(wk[0:64, 0, :n], ps1[0:64, :n], tb[:, 0, :n], ALU.mult), r=[pk1, tk], w=['wk'])
                ps2, pk2 = pp.next()
                for kk in range(QLc):
                    st.mm(ps2[0:64, :n], wuq[:, kk, 192:256], cn[:, kk, :n], kk == 0, kk == QLc - 1, r=['cn', 'wuq'], w=[pk2])
                st.dve(lambda e, ps2=ps2, tb=tb, n=n: e.tensor_tensor(wk[0:64, 1, :n], ps2[0:64, :n], tb[:, 1, :n], ALU.mult), r=[pk2, tk], w=['wk'])
                st.dve(lambda e, n=n, t0=t0: e.tensor_tensor(QR[:, t0:t0 + n], wk[0:64, 0, :n], wk[0:64, 1, :n], ALU.add), r=['wk'], w=['QR'])
                for tb_i in range(n // 128):
                    ps, pk = pp.next()
                    for kk in range(KVc):
                        st.mm(ps[:, :128], cn[:, QLc + kk, tb_i * 128:(tb_i + 1) * 128], wukv[:, kk, 128:256], kk == 0, kk == KVc - 1, r=['cn', 'wukv'], w=[pk])
                    st.act(V[:, t0 // 128 + tb_i, :], ps[:, :128], AF.Copy, r=[pk], w=['V'])
            run_attention(ac, [(lambda q0, n: QN[:, q0:q0 + n], 'QN'), (lambda q0, n: QR[:, q0:q0 + n], 'QR')],
                          [(lambda kc: KN[:, kc * 128:(kc + 1) * 128], 'KN'), (lambda kc: KR[:, kc * 128:(kc + 1) * 128], 'KR')],
                          lambda kc: (V[:, kc, :], 'V'), 192.0 ** -0.5, (0 * H + hh) * 128, need_ctx, [])
            st.run()

    def stage_3c(l, need_ctx):
        rows = S // 64
        WH_ = c['WH']
        for hh in range(H):
            st = Stage(nc, f"S3C_{l}_{hh}")
            ac = AttnCtx(st)
            bias = st.sb([128, WH_, 256], F32, 'bias')
            st.dma(bias[:], I[f'nab{l}'][hh * WH_ * 128:(hh + 1) * WH_ * 128, :].rearrange("(a p) c -> p a c", p=128), r=[], w=['bias'])
            QT = st.sb([128, TB], BF16, 'QT'); KT = st.sb([128, TB], BF16, 'KT')
            oq, ok2 = SEG['nq'][0] + hh * 128, SEG['nk'][0] + hh * 128
            st.dma(QT[:], projT[oq:oq + 128, :], r=['projT'], w=['QT'])
            st.dma(KT[:], projT[ok2:ok2 + 128, :], r=['projT'], w=['KT'])
            Va = st.sb([128, S // 128, 128], BF16, 'Va'); Vb = st.sb([128, S // 128, 128], BF16, 'Vb'); Vc = st.sb([128, CKC, 128], BF16, 'Vc')
            nvh = nvtok[:, hh * 128:(hh + 1) * 128]
            vav = nvh[CTX:CTX + S, :].rearrange("(c p) d -> p c d", p=128)
            vbv = nvh[CTX + 64:CTX + 64 + S - 128, :].rearrange("(c p) d -> p c d", p=128)
            for c0_ in range(0, S // 128, 16):
                c1_ = min(S // 128, c0_ + 16)
                st.dma(Va[:, c0_:c1_, :], vav[:, c0_:c1_, :], r=['vtok'], w=['Va'])
                c1b = min(S // 128 - 1, c0_ + 16)
                if c1b > c0_:
                    st.dma(Vb[:, c0_:c1b, :], vbv[:, c0_:c1b, :], r=['vtok'], w=['Vb'])
            st.dma(Vc[:], nvh[0:CTX, :].rearrange("(c p) d -> p c d", p=128), r=['vtok'], w=['Vc'])
            tmpr = Ring([(st.sb([128, 256], F32, 'tmp'), f'tmp{i}') for i in range(2)])
            scale = 128.0 ** -0.5
            ncol = 256 + 64 * CKC
            pendrow = None
            for g in range(rows // 8):
                O, ok_ = ac.op.next()
                M, mk = ac.mp.next()
                for rr in range(8):
                    r = g * 8 + rr
                    rs_ = min(max(r - WH_ // 2, 0), rows - WH_)
                    v = r - rs_
                    q0 = CTX + 64 * r
                    k0 = CTX + 64 * rs_
                    Sp, sk = ac.sp.next()
                    for i4 in range(4):
                        st.mm(Sp[:, i4 * 64:(i4 + 1) * 64], KT[:, k0 + i4 * 128:k0 + (i4 + 1) * 128], QT[:, q0:q0 + 64], True, True, r=['KT', 'QT'], w=[sk])
                    for cc in range(CKC):
                        st.mm(Sp[:, 256 + cc * 64:256 + (cc + 1) * 64], KT[:, cc * 128:(cc + 1) * 128], QT[:, q0:q0 + 64], True, True, r=['KT', 'QT'], w=[sk])
                    tmp, tk = tmpr.next()
                    st.dve(lambda e, Sp=Sp, tmp=tmp, v=v: e.scalar_tensor_tensor(tmp[:], Sp[:, :256], scale, bias[:, v, :], ALU.mult, ALU.add), r=[sk, 'bias'], w=[tk])
                    PT, pk = ac.pt.next()
                    st.act(PT[:, :256], tmp[:], AF.Exp, r=[tk], w=[pk])
                    st.act(PT[:, 256:ncol], Sp[:, 256:ncol], AF.Exp, r=[sk], w=[pk], scale=scale)
                    def pvrow(rr=rr, rs_=rs_, PT=PT, pk=pk):
                        for idx in range(4 + CKC):
                            if idx < 4:
                                va = Va[:, rs_ // 2 + idx, :] if rs_ % 2 == 0 else Vb[:, (rs_ - 1) // 2 + idx, :]
                                vk = 'Va' if rs_ % 2 == 0 else 'Vb'
                            else:
                                va = Vc[:, idx - 4, :]; vk = 'Vc'
                            st.mm(O[:, rr * 64:(rr + 1) * 64], va, PT[:, idx * 64:(idx + 1) * 64], idx == 0, idx == 3 + CKC, r=[pk, vk], w=[ok_])
                            st.mm(M[:, rr * 64:(rr + 1) * 64], ac.ones[:], PT[:, idx * 64:(idx + 1) * 64], idx == 0, idx == 3 + CKC, r=[pk, 'ones'], w=[mk])
                    if pendrow is not None:
                        pendrow()
                    pendrow = pvrow
                pendrow()
                pendrow = None
                rs, rkx = ac.rs.next()
                st.dve(lambda e, rs=rs, M=M: e.reciprocal(rs[:], M[:]), r=[mk], w=[rkx])
                y, yk = ac.y.next()
                st.dve(lambda e, y=y, O=O, rs=rs: e.tensor_tensor(y[:], O[:], rs[:], ALU.mult), r=[ok_, rkx], w=[yk])
                yr = (2 * H + hh) * 128
                st.dma(yT_sh[yr:yr + 128, CTX + 512 * g:CTX + 512 * (g + 1)], y[:], r=[yk], w=['yT_sh'], q='pool')
            if need_ctx:
                for t0 in range(0, CTX, 512):
                    attention(ac, [(lambda q0, n: QT[:, q0:q0 + n], 'QT')], [(lambda kc: KT[:, kc * 128:(kc + 1) * 128], 'KT')],
                              lambda kc: (Vc[:, kc, :], 'Vc'), list(range(CKC)), t0, min(512, CTX - t0), scale, (2 * H + hh) * 128, [])
            st.run()

    def stage_3d(l, need_ctx):
        for hh in range(H):
            st = Stage(nc, f"S3D_{l}_{hh}")
            x = st.sb([128, TB], BF16, 'x')
            ox, og = SEG['lx'][0] + hh * 128, SEG['lg'][0] + hh * 128
            st.dma(x[:], projT[ox:ox + 128, :], r=['projT'], w=['x'])
            cw = st.sb([128, 4], F32, 'cw'); cb = st.sb([128, 1], F32, 'cb')
            st.dma(cw[:], I[f'lcw{l}'][:, hh * 4:(hh + 1) * 4], r=[], w=['cw'])
            st.dma(cb[:], I[f'lcb{l}'][:, hh:hh + 1], r=[], w=['cb'], allow_slow_non_contiguous=True)
            wa = st.sb([128, 2, 128], BF16, 'wa'); wx = st.sb([128, 2, 128], BF16, 'wx')
            pv = st.sb([128, 3, 2], F32, 'pv')
            for d in range(2):
                cs_ = (d * H + hh) * 128
                st.dma(wa[:, d, :], I[f'lwa{l}'][:, cs_:cs_ + 128], r=[], w=['wa'], q='pool')
                st.dma(wx[:, d, :], I[f'lwx{l}'][:, cs_:cs_ + 128], r=[], w=['wx'], q='pool')
                for pi, nm in enumerate(('lba', 'lbx', 'llam')):
                    st.dma(pv[:, pi, d:d + 1], I[f'{nm}{l}'][:, d * H + hh:d * H + hh + 1], r=[], w=['pv'], allow_slow_non_contiguous=True)
            nsp = st.sb([128, 2], F32, 'nsp')
            st.act(nsp[:], pv[:, 2, :], AF.Exp, r=['pv'], w=['nsp'], scale=-1.0)
            st.act(nsp[:], nsp[:], AF.Ln, r=['nsp'], w=['nsp'], bias=1.0)
            st.dve(lambda e: e.tensor_scalar(nsp[:], nsp[:], -8.0, None, ALU.mult), r=['nsp'], w=['nsp'])
            xc = st.sb([128, TB], F32, 'xc'); xcb = st.sb([128, TB], BF16, 'xcb'); hs = st.sb([128, TB], F32, 'hs')
            for (a, b) in ((0, CTX), (CTX, TB)):
                st.dve(lambda e, a=a, b=b: e.tensor_scalar(xc[:, a:b], x[:, a:b], cw[:, 2:3], cb[:, 0:1], ALU.mult, ALU.add), r=['x', 'cw', 'cb'], w=['xc'])
                st.dve(lambda e, a=a, b=b: e.scalar_tensor_tensor(xc[:, a + 2:b], x[:, a:b - 2], cw[:, 0:1], xc[:, a + 2:b], ALU.mult, ALU.add), r=['x', 'cw', 'xc'], w=['xc'])
                st.dve(lambda e, a=a, b=b: e.scalar_tensor_tensor(xc[:, a + 1:b], x[:, a:b - 1], cw[:, 1:2], xc[:, a + 1:b], ALU.mult, ALU.add), r=['x', 'cw', 'xc'], w=['xc'])
                st.dve(lambda e, a=a, b=b: e.scalar_tensor_tensor(xc[:, a:b - 1], x[:, a + 1:b], cw[:, 3:4], xc[:, a:b - 1], ALU.mult, ALU.add), r=['x', 'cw', 'xc'], w=['xc'])
            st.dve(lambda e: e.tensor_copy(xcb[:], xc[:]), r=['xc'], w=['xcb'])
            pp = Ring([(st.ps(), f'pp{i}') for i in range(4)])
            wr_ = Ring([(st.sb([128, 4, 512], F32, 'w'), f'w{i}') for i in range(2)])
            hb = st.sb([128, 512], F32, 'hb')
            carry = st.sb([128, 1], F32, 'carry')
            ctiles = [t for t in batch_tiles(True) if t['ctx']]
            ltiles = [t for t in batch_tiles(True) if not t['ctx']]
            for d in range(2):
                order = (ctiles + ltiles) if d == 0 else (ctiles[::-1] + ltiles[::-1])
                for ti, tile in enumerate(order):
                    t0, n = tile['t0'], tile['n']
                    w_, wk = wr_.next()
                    pr_, pkr = pp.next(); pi_, pki = pp.next()
                    st.mm(pr_[:, :n], wa[:, d, :], xcb[:, t0:t0 + n], True, True, r=['wa', 'xcb'], w=[pkr])
                    st.mm(pi_[:, :n], wx[:, d, :], xcb[:, t0:t0 + n], True, True, r=['wx', 'xcb'], w=[pki])
                    st.act(w_[:, 0, :n], pr_[:, :n], AF.Sigmoid, r=[pkr, 'pv'], w=[wk], bias=pv[:, 0, d:d + 1])
                    st.act(w_[:, 1, :n], pi_[:, :n], AF.Sigmoid, r=[pki, 'pv'], w=[wk], bias=pv[:, 1, d:d + 1])
                    st.act(w_[:, 0, :n], w_[:, 0, :n], AF.Exp, r=[wk, 'nsp'], w=[wk], scale=nsp[:, d:d + 1])
                    st.dve(lambda e, w_=w_, n=n: e.tensor_tensor(w_[:, 2, :n], w_[:, 0, :n], w_[:, 0, :n], ALU.mult), r=[wk], w=[wk])
                    st.dve(lambda e, w_=w_, n=n: e.tensor_scalar(w_[:, 2, :n], w_[:, 2, :n], -1.0, 1.0, ALU.mult, ALU.add), r=[wk], w=[wk])
                    st.act(w_[:, 2, :n], w_[:, 2, :n], AF.Sqrt, r=[wk], w=[wk])
                    st.dve(lambda e, w_=w_, n=n: e.tensor_tensor(w_[:, 1, :n], w_[:, 1, :n], w_[:, 2, :n], ALU.mult), r=[wk], w=[wk])
                    st.dve(lambda e, w_=w_, n=n, t0=t0: e.tensor_tensor(w_[:, 1, :n], w_[:, 1, :n], xc[:, t0:t0 + n], ALU.mult), r=[wk, 'xc'], w=[wk])
                    if d == 0:
                        init = 0.0 if t0 == 0 else hs[:, t0 - 1:t0]
                        st.dve(lambda e, w_=w_, n=n, t0=t0, init=init: e.tensor_tensor_scan(hs[:, t0:t0 + n], w_[:, 0, :n], w_[:, 1, :n], init, ALU.mult, ALU.add), r=[wk, 'hs'], w=['hs'])
                    else:
                        init = 0.0 if ti == 0 else carry[:, 0:1]
                        st.dve(lambda e, w_=w_, n=n, init=init: e.tensor_tensor_scan(hb[:, 0:n][:, ::-1], w_[:, 0, 0:n][:, ::-1], w_[:, 1, 0:n][:, ::-1], init, ALU.mult, ALU.add), r=[wk, 'carry'], w=['hb'])
                        st.dve(lambda e: e.tensor_copy(carry[:], hb[:, 0:1]), r=['hb'], w=['carry'])
                        st.dve(lambda e, n=n, t0=t0: e.tensor_tensor(hs[:, t0:t0 + n], hs[:, t0:t0 + n], hb[:, :n], ALU.add), r=['hb', 'hs'], w=['hs'])
            gr = Ring([(st.sb([128, 512], BF16, 'g'), f'g{i}') for i in range(2)])
            ur = Ring([(st.sb([128, 2, 512], F32, 'u'), f'u{i}') for i in range(2)])
            yr_ = Ring([(st.sb([128, 512], BF16, 'y'), f'y{i}') for i in range(2)])
            for tile in batch_tiles(need_ctx):
                t0, n = tile['t0'], tile['n']
                g, gk = gr.next(); u, uk = ur.next(); y, yk = yr_.next()
                st.dma(g[:, :n], projT[og:og + 128, t0:t0 + n], r=['projT'], w=[gk])
                st.dve(lambda e, g=g, u=u, n=n: e.tensor_tensor(u[:, 0, :n], g[:, :n], g[:, :n], ALU.mult), r=[gk], w=[uk])
                st.dve(lambda e, u=u, n=n: e.tensor_scalar(u[:, 0, :n], u[:, 0, :n], 0.044715, 1.0, ALU.mult, ALU.add), r=[uk], w=[uk])
                st.dve(lambda e, g=g, u=u, n=n: e.tensor_tensor(u[:, 0, :n], u[:, 0, :n], g[:, :n], ALU.mult), r=[uk, gk], w=[uk])
                st.act(u[:, 0, :n], u[:, 0, :n], AF.Tanh, r=[uk], w=[uk], scale=0.7978845608028654)
                st.dve(lambda e, u=u, n=n: e.tensor_scalar(u[:, 0, :n], u[:, 0, :n], 0.5, 0.5, ALU.mult, ALU.add), r=[uk], w=[uk])
                st.dve(lambda e, g=g, u=u, n=n: e.tensor_tensor(u[:, 0, :n], u[:, 0, :n], g[:, :n], ALU.mult), r=[uk, gk], w=[uk])
                st.dve(lambda e, u=u, y=y, n=n, t0=t0: e.tensor_tensor(y[:, :n], u[:, 0, :n], hs[:, t0:t0 + n], ALU.mult), r=[uk, 'hs'], w=[yk])
                yr0 = (3 * H + hh) * 128
                st.dma(yT_sh[yr0:yr0 + 128, t0:t0 + n], y[:, :n], r=[yk], w=['yT_sh'], q='pool')
            st.run()

    yT_g = scr("yT_g", [NCORE * 4 * H * 128, TB])

    def stage_4(l):
        gather_select(f"S4_{l}", yT_sh, yT_g, yT_all, 4 * H * 128, TB, 1)

    mT = scr("mT", [D, TOK])
    h2T = scr("h2T", [D, TOK])
    gates_d = scr("gates_d", [TOK, NE], F32)
    NCH = CPB * 4 * H

    def own_tcols(tile, jj):
        if tile['ctx']:
            return jj * TC
        return CTX + jj * TL + (tile['col0'] - TC)

    def stage_5a(l, need_ctx):
        st = Stage(nc, f"S5A_{l}")
        oh = st.sb([128, CPB], F32, 'oh')
        st.dma(oh[:], I['oh'], r=[], w=['oh'])
        bb = st.sb([128, 4 * KC], F32, 'bb')
        st.dma(bb[:], I[f'bbg{l}'], r=[], w=['bb'])
        hr = Ring([(st.sb([128, KC, 512], BF16, 'h'), f'h{i}') for i in range(1)])
        ys = st.sb([128, NCH, 512], BF16, 'ys')
        yl = Ring([(st.sb([128, NCH // 2, 512], BF16, 'yl'), f'yl{i}') for i in range(2)])
        wgr = Ring([(st.sb([128, KC, 128], BF16, 'wg'), f'wg{i}') for i in range(3)])
        wbr = Ring([(st.sb([128, NH, 128], BF16, 'wb'), f'wb{i}') for i in range(3)])
        mg = st.sb([128, KC, 512], BF16, 'mg')
        Gr = Ring([(st.sb([128, 512], F32, 'G'), f'G{i}') for i in range(2)])
        acc = st.sb([128, 2, 512], F32, 'acc')
        pg = Ring([(st.ps(), f'pg{i}') for i in range(3)])
        pt = Ring([(st.ps(), f'pt{i}') for i in range(3)])
        hv = hT_sh.rearrange("(k p) t -> p k t", p=128)
        yv = yT_all.rearrange("(a p) t -> p a t", p=128)
        wgv, KG = wview('wbg', l)
        wbv, KB = wview('wb', l)
        SPG = NCORE // 4
        mv = mT.rearrange("(k p) t -> p k t", p=128)
        for tile in own_tiles(need_ctx):
            n, col0 = tile['n'], tile['col0']
            ht, hk = hr.next()
            st.dma(ht[:, :, :n], hv[:, :, col0:col0 + n], r=['hT_sh'], w=[hk])
            for half in range(2):
                a0 = half * (NCH // 2)
                for jj in range(CPB):
                    yt, yk = yl.next()
                    tc0 = own_tcols(tile, jj)
                    st.dma(yt[:, :, :n], yv[:, a0:a0 + NCH // 2, tc0:tc0 + n], r=['yT_all'], w=[yk])
                    if jj == 0:
                        st.dve(lambda e, yt=yt, a0=a0, n=n: e.tensor_scalar(ys[:, a0:a0 + NCH // 2, :n], yt[:, :, :n], oh[:, 0:1], None, ALU.mult), r=[yk, 'oh'], w=['ys'])
                    else:
                        st.dve(lambda e, yt=yt, a0=a0, n=n, jj=jj: e.scalar_tensor_tensor(ys[:, a0:a0 + NCH // 2, :n], yt[:, :, :n], oh[:, jj:jj + 1], ys[:, a0:a0 + NCH // 2, :n], ALU.mult, ALU.add), r=[yk, 'oh', 'ys'], w=['ys'])
            for fc in range(KC):
                for i in range(4):
                    wg, wgk = wgr.next()
                    for sp_ in range(SPG):
                        st.dma(wg[:, sp_ * KG:(sp_ + 1) * KG, :], wgv[i * SPG + sp_, 0, fc], r=['wbg'], w=[wgk])
                    wb, wbk = wbr.next()
                    for sp_ in range(SPG):
                        st.dma(wb[:, sp_ * KB:(sp_ + 1) * KB, :], wbv[i * SPG + sp_, 0, fc], r=['wb'], w=[wbk])
                    Gp, gpk = pg.next()
                    for kk in range(KC):
                        st.mm(Gp[:, :n], wg[:, kk, :], ht[:, kk, :n], kk == 0, kk == KC - 1, r=[wgk, hk], w=[gpk])
                    Tp, tpk = pt.next()
                    for hd in range(NH):
                        ch = (hd // H) * 4 * H + i * H + (hd % H)
                        st.mm(Tp[:, :n], wb[:, hd, :], ys[:, ch, :n], hd == 0, hd == NH - 1, r=[wbk, 'ys'], w=[tpk])
                    G, gk = Gr.next()
                    st.act(G[:, :n], Gp[:, :n], AF.Sigmoid, r=[gpk, 'bb'], w=[gk], bias=bb[:, i * KC + fc:i * KC + fc + 1])
                    if i == 0:
                        st.dve(lambda e, G=G, Tp=Tp, n=n: e.tensor_tensor(acc[:, 0, :n], G[:, :n], Tp[:, :n], ALU.mult), r=[gk, tpk], w=['acc'])
                    else:
                        st.dve(lambda e, G=G, Tp=Tp, n=n: e.tensor_tensor(acc[:, 1, :n], G[:, :n], Tp[:, :n], ALU.mult), r=[gk, tpk], w=['acc'])
                        if i < 3:
                            st.dve(lambda e, n=n: e.tensor_tensor(acc[:, 0, :n], acc[:, 0, :n], acc[:, 1, :n], ALU.add), r=['acc'], w=['acc'])
                        else:
                            st.dve(lambda e, n=n, fc=fc: e.tensor_tensor(mg[:, fc, :n], acc[:, 0, :n], acc[:, 1, :n], ALU.add), r=['acc'], w=['mg'])
            st.dma(mv[:, :, col0:col0 + n], mg[:, :, :n], r=['mg'], w=['mT'], q='pool')
        st.run()

    def load_gate_bc(st, l, m):
        gb = st.sb([128, 2, D], F32, 'gb')
        for i in range(2):
            st.dma(gb[:, i, :], modown[l][i:i + 1, m * D:(m + 1) * D].partition_broadcast(128), r=[], w=['gb'])
        return gb

    def stage_5b(l, need_ctx):
        st = Stage(nc, f"S5B_{l}")
        gb = load_gate_bc(st, l, 2)
        mr = Ring([(st.sb([128, KC, 512], BF16, 'm'), f'm{i}') for i in range(1)])
        wor = Ring([(st.sb([128, KC, 512], BF16, 'wo'), f'wo{i}') for i in range(2)])
        xr = Ring([(st.sb([128, 512], F32, 'x'), f'x{i}') for i in range(3)])
        pr = Ring([(st.ps(), f'p{i}') for i in range(4)])
        mv = mT.rearrange("(k p) t -> p k t", p=128)
        wov, KO = wview('wout', l)
        which = 'in' if l == 0 else 'res'
        for tile in own_tiles(need_ctx):
            n, col0 = tile['n'], tile['col0']
            i = 1 if tile['ctx'] else 0
            mt_, mk = mr.next()
            st.dma(mt_[:, :, :n], mv[:, :, col0:col0 + n], r=['mT'], w=[mk])
            for fo in range(D // 512):
                wo, wok = wor.next()
                for r_ in range(NCORE):
                    st.dma(wo[:, r_ * KO:(r_ + 1) * KO, :], wov[r_, 0, fo], r=['wout'], w=[wok])
                for (r0, nt) in tile['blocks']:
                    b0 = r0 - col0
                    ps, pk = pr.next()
                    for kk in range(KC):
                        st.mm(ps[:nt, :], mt_[:, kk, b0:b0 + nt], wo[:, kk, :], kk == 0, kk == KC - 1, r=[mk, wok], w=[pk])
                    xt, xk = xr.next()
                    st.dma(xt[:nt, :], xsrc_rows(l, r0, nt, which)[:, fo * 512:(fo + 1) * 512], r=[], w=[xk])
                    st.dve(lambda e, ps=ps, xt=xt, nt=nt, fo=fo, i=i: e.tensor_tensor(ps[:nt, :], ps[:nt, :], gb[:nt, i, fo * 512:(fo + 1) * 512], ALU.mult), r=[pk, 'gb'], w=[pk])
                    st.dve(lambda e, ps=ps, xt=xt, nt=nt: e.tensor_tensor(xt[:nt, :], xt[:nt, :], ps[:nt, :], ALU.add), r=[pk, xk], w=[xk])
                    st.dma(xmid[r0:r0 + nt, fo * 512:(fo + 1) * 512], xt[:nt, :], r=[xk], w=['xmid'], q='pool')
        st.run()

    def stage_6a(l, need_ctx):
        st = Stage(nc, f"S6A_{l}")
        A, mt = load_mod(st, l, 'nffn', 3, 4)
        wrt = st.sb([128, KC * NE], F32, 'wrt')
        st.dma(wrt[:], I['wr'], r=[], w=['wrt'])
        rbt = st.sb([128, NE], F32, 'rbt')
        st.dma(rbt[:], I['rb'].partition_broadcast(128), r=[], w=['rbt'])
        hfr = Ring([(st.sb([128, 128], F32, 'hf'), f'hf{i}') for i in range(3)])
        lp = Ring([(st.ps(), f'lp{i}') for i in range(2)])
        R_ = Ring([(st.sb([128, 12, NE], F32, 'R'), f'R{i}') for i in range(2)])
        hv = h2T.rearrange("(k p) t -> p k t", p=128)
        cur = {}

        def want(st, tile, bi, kk, ps_ap, pk, A_ap, S_ap, nt):
            if kk == 0:
                cur['lp'] = lp.next()
            L, lk = cur['lp']
            hf, hfk = hfr.next()
            st.act(hf[:, :nt], ps_ap, AF.Identity, r=[pk, 'A', 'mt'], w=[hfk], bias=S_ap, scale=A_ap)
            st.mm(L[:nt, :NE], hf[:, :nt], wrt[:, kk * NE:(kk + 1) * NE], kk == 0, kk == KC - 1, r=[hfk, 'wrt'], w=[lk])
            if kk == KC - 1:
                r0 = tile['blocks'][bi][0]
                R, rk = R_.next()
                BIG = 1.0e4
                X = lambda a: R[:nt, a, :]
                G4 = lambda a: R[:nt, a, :].rearrange("p (g e) -> p g e", e=4)
                d = lambda fn, rr=[rk], ww=[rk]: st.dve(fn, r=rr, w=ww)
                st.act(X(0), L[:nt, :NE], AF.Sigmoid, r=[lk], w=[rk])
                d(lambda e: e.tensor_tensor(X(1), X(0), rbt[:nt, :], ALU.add), [rk, 'rbt'])
                d(lambda e: e.tensor_reduce(R[:nt, 2, 0:4], G4(1), AX.X, ALU.max))
                d(lambda e: e.tensor_tensor(G4(3), G4(1), R[:nt, 2, 0:4].unsqueeze(2).to_broadcast([nt, 4, 4]), ALU.is_equal))
                d(lambda e: e.scalar_tensor_tensor(X(3), X(3), -BIG, X(1), ALU.mult, ALU.add))
                d(lambda e: e.tensor_reduce(R[:nt, 2, 4:8], G4(3), AX.X, ALU.max))
                d(lambda e: e.tensor_tensor(R[:nt, 2, 8:12], R[:nt, 2, 0:4], R[:nt, 2, 4:8], ALU.add))
                d(lambda e: e.tensor_reduce(R[:nt, 2, 12:13], R[:nt, 2, 8:12], AX.X, ALU.max))
                d(lambda e: e.tensor_tensor(R[:nt, 4, 0:4], R[:nt, 2, 8:12], R[:nt, 2, 12:13].to_broadcast([nt, 4]), ALU.is_equal))
                d(lambda e: e.tensor_scalar(R[:nt, 4, 0:4], R[:nt, 4, 0:4], BIG, -BIG, ALU.mult, ALU.add))
                d(lambda e: e.tensor_tensor(G4(5), G4(1), R[:nt, 4, 0:4].unsqueeze(2).to_broadcast([nt, 4, 4]), ALU.add))
                d(lambda e: e.tensor_reduce(R[:nt, 2, 13:14], X(5), AX.X, ALU.max))
                d(lambda e: e.tensor_tensor(X(6), X(5), R[:nt, 2, 13:14].to_broadcast([nt, NE]), ALU.is_equal))
                d(lambda e: e.scalar_tensor_tensor(X(7), X(6), -BIG, X(5), ALU.mult, ALU.add))
                d(lambda e: e.tensor_reduce(R[:nt, 2, 14:15], X(7), AX.X, ALU.max))
                d(lambda e: e.tensor_tensor(X(8), X(7), R[:nt, 2, 14:15].to_broadcast([nt, NE]), ALU.is_equal))
                d(lambda e: e.tensor_tensor(X(6), X(6), X(8), ALU.add))
                d(lambda e: e.tensor_tensor(X(6), X(6), X(0), ALU.mult))
                d(lambda e: e.tensor_reduce(R[:nt, 2, 15:16], X(6), AX.X, ALU.add))
                d(lambda e: e.reciprocal(R[:nt, 9, 0:1], R[:nt, 2, 15:16]))
                d(lambda e: e.tensor_scalar(X(10), X(6), R[:nt, 9, 0:1], None, ALU.mult))
                st.dma(gates_d[r0:r0 + nt, :], X(10), r=[rk], w=['gates'], q='pool')

        def consume(st, tile, hT, hk):
            st.dma(hv[:, :, tile['col0']:tile['col0'] + tile['n']], hT[:, :, :tile['n']], r=[hk], w=['h2T'], q='pool')

        norm_tiles(st, l, 'mid', A, mt, 3, own_tiles(need_ctx), consume, want_f32=want)
        st.run()

    def stage_6b(l, need_ctx):
        st = Stage(nc, f"S6B_{l}")
        DEc = DE // 128
        hr = Ring([(st.sb([128, KC, 512], BF16, 'h'), f'h{i}') for i in range(1)])
        yacc = st.sb([128, 4, D], F32, 'yacc')
        gt = st.sb([128, 4, NE], F32, 'gt')
        wgr = Ring([(st.sb([128, KC, 128], BF16, 'wg'), f'wg{i}') for i in range(4)])
        wdr = Ring([(st.sb([128, DEc, 512], BF16, 'wd'), f'wd{i}') for i in range(2)])
        he = st.sb([128, DEc, 512], BF16, 'he')
        tr_ = Ring([(st.sb([128, 512], F32, 't'), f't{i}') for i in range(2)])
        gbr = Ring([(st.sb([128, 512], F32, 'gb'), f'gb{i}') for i in range(2)])
        xr = Ring([(st.sb([128, 512], F32, 'x'), f'x{i}') for i in range(2)])
        pg = Ring([(st.ps(), f'pg{i}') for i in range(2)])
        pu = Ring([(st.ps(), f'pu{i}') for i in range(2)])
        pd = Ring([(st.ps(), f'pd{i}') for i in range(3)])
        hv = h2T.rearrange("(k p) t -> p k t", p=128)
        wgv, _ = wview('weg', l)
        wuv, _ = wview('weu', l)
        wdv, _ = wview('wed', l)
        EPS = NE // NCORE
        for tile in own_tiles(need_ctx):
            n, col0 = tile['n'], tile['col0']
            i = 1 if tile['ctx'] else 0
            ht, hk = hr.next()
            st.dma(ht[:, :, :n], hv[:, :, col0:col0 + n], r=['h2T'], w=[hk])
            for bi, (r0, nt) in enumerate(tile['blocks']):
                st.dma(gt[:nt, bi, :], gates_d[r0:r0 + nt, :], r=['gates'], w=['gt'])
            for ex in range(NE):
                for j in range(DEc):
                    wg, wgk = wgr.next()
                    st.dma(wg[:], wgv[ex // EPS, ex % EPS, j], r=['weg'], w=[wgk])
                    wu, wuk = wgr.next()
                    st.dma(wu[:], wuv[ex // EPS, ex % EPS, j], r=['weu'], w=[wuk])
                    Hg, hgk = pg.next(); Hu, huk = pu.next()
                    for kk in range(KC):
                        st.mm(Hg[:, :n], wg[:, kk, :], ht[:, kk, :n], kk == 0, kk == KC - 1, r=[wgk, hk], w=[hgk])
                    for kk in range(KC):
                        st.mm(Hu[:, :n], wu[:, kk, :], ht[:, kk, :n], kk == 0, kk == KC - 1, r=[wuk, hk], w=[huk])
                    t, tk = tr_.next()
                    st.act(t[:, :n], Hg[:, :n], AF.Silu, r=[hgk], w=[tk])
                    st.dve(lambda e, t=t, Hu=Hu, j=j, n=n: e.tensor_tensor(he[:, j, :n], t[:, :n], Hu[:, :n], ALU.mult), r=[tk, huk], w=['he'])
                for fo in range(D // 512):
                    wd, wdk = wdr.next()
                    st.dma(wd[:], wdv[ex // EPS, ex % EPS, fo], r=['wed'], w=[wdk])
                    for bi, (r0, nt) in enumerate(tile['blocks']):
                        b0 = r0 - col0
                        ps, pk = pd.next()
                        for j in range(DEc):
                            st.mm(ps[:nt, :], he[:, j, b0:b0 + nt], wd[:, j, :], j == 0, j == DEc - 1, r=['he', wdk], w=[pk])
                        ya = yacc[:nt, bi, fo * 512:(fo + 1) * 512]
                        if ex == 0:
                            st.dve(lambda e, ps=ps, ya=ya, nt=nt, bi=bi, ex=ex: e.tensor_scalar(ya, ps[:nt, :], gt[:nt, bi, ex:ex + 1], None, ALU.mult), r=[pk, 'gt'], w=['yacc'])
                        else:
                            st.dve(lambda e, ps=ps, ya=ya, nt=nt, bi=bi, ex=ex: e.scalar_tensor_tensor(ya, ps[:nt, :], gt[:nt, bi, ex:ex + 1], ya, ALU.mult, ALU.add), r=[pk, 'gt', 'yacc'], w=['yacc'])
            for fo in range(D // 512):
                gb, gbk = gbr.next()
                st.dma(gb[:], modown[l][i:i + 1, 5 * D + fo * 512:5 * D + (fo + 1) * 512].partition_broadcast(128), r=[], w=[gbk])
                for bi, (r0, nt) in enumerate(tile['blocks']):
                    xt, xk = xr.next()
                    st.dma(xt[:nt, :], xmid[r0:r0 + nt, fo * 512:(fo + 1) * 512], r=['xmid'], w=[xk])
                    ya = yacc[:nt, bi, fo * 512:(fo + 1) * 512]
                    st.dve(lambda e, ya=ya, gb=gb, nt=nt: e.tensor_tensor(ya, ya, gb[:nt, :], ALU.mult), r=['yacc', gbk], w=['yacc'])
                    st.dve(lambda e, ya=ya, xt=xt, nt=nt: e.tensor_tensor(xt[:nt, :], xt[:nt, :], ya, ALU.add), r=['yacc', xk], w=[xk])
                    st.dma(xres[r0:r0 + nt, fo * 512:(fo + 1) * 512], xt[:nt, :], r=[xk], w=['xres'], q='pool')
        st.run()

    def stage_7():
        st = Stage(nc, "S7")
        fn = st.sb([128, D], F32, 'fn')
        st.dma(fn[:], I['fnorm'].partition_broadcast(128), r=[], w=['fn'])
        xr = Ring([(st.sb([128, D], F32, 'x'), f'x{i}') for i in range(2)])
        junk = st.sb([128, D], BF16, 'junk')
        ssr = Ring([(st.sb([128, 4], F32, 'ss'), f'ss{i}') for i in range(2)])
        for r0 in range(TC, TOK, 128):
            xt, xk = xr.next(); ss, sk = ssr.next()
            st.dma(xt[:], xres[r0:r0 + 128, :], r=[], w=[xk])
            st.add('act', (lambda e, xt=xt, ss=ss: e.activation(junk[:], xt[:], AF.Square, accum_out=ss[:, 0:1])), [xk], ['junk', sk])
            st.dve(lambda e, ss=ss: e.tensor_scalar(ss[:, 1:2], ss[:, 0:1], 1.0 / D, 1e-6, ALU.mult, ALU.add), r=[sk], w=[sk])
            st.act(ss[:, 3:4], ss[:, 1:2], AF.Sqrt, r=[sk], w=[sk])
            st.dve(lambda e, ss=ss: e.reciprocal(ss[:, 2:3], ss[:, 3:4]), r=[sk], w=[sk])
            st.act(xt[:], xt[:], AF.Copy, r=[xk, sk], w=[xk], scale=ss[:, 2:3])
            st.dve(lambda e, xt=xt: e.tensor_tensor(xt[:], xt[:], fn[:], ALU.mult), r=[xk, 'fn'], w=[xk])
            st.dma(out_own[r0 - TC:r0 - TC + 128, :], xt[:], r=[xk], w=['out'], q='pool')
        st.run()

    def build_all():
        for l in range(c['DEPTH']):
            need_ctx = l < c['DEPTH'] - 1
            stage_w(l); stage_m(l); stage_1(l); stage_2(l)
            stage_3a(l, need_ctx); stage_3b(l, need_ctx); stage_3c(l, need_ctx); stage_3d(l, need_ctx); stage_4(l)
            stage_5a(l, need_ctx); stage_5b(l, need_ctx); stage_6a(l, need_ctx); stage_6b(l, need_ctx)
        stage_7()

    def stage_dbg(src, name):
        dst = nc.dram_tensor(name, list(src.shape), src.dtype, kind="ExternalOutput").ap()
        st = Stage(nc, f"DBG{name}")
        st.dma(dst, src, r=[], w=['d'])
        st.run()

    k.nc = nc
    k.stages = dict(w=stage_w, m=stage_m, s1=stage_1, s2=stage_2, s3a=stage_3a, s3b=stage_3b, s3c=stage_3c, s3d=stage_3d, s4=stage_4, s5a=stage_5a, s5b=stage_5b, s6a=stage_6a, s6b=stage_6b, s7=stage_7, all=build_all, dbg=stage_dbg)
    k.locals = locals()
    return k


def rope_tables(c, rot):
    S, CTX, GW = c['S'], c['CTX'], c['GRID_W']
    t = np.arange(S, dtype=np.int32)
    row = (t // GW).astype(np.float32)
    col = (t % GW).astype(np.float32)
    nf = rot // 4
    freqs = (np.float32(10000.0) ** (-np.arange(nf, dtype=np.float32) / np.float32(nf))).astype(np.float32)
    ang = np.concatenate([row[:, None] * freqs, col[:, None] * freqs], axis=-1).astype(np.float32)
    cs, sn = np.cos(ang).astype(np.float32), np.sin(ang).astype(np.float32)
    C = np.ones((rot, CTX + S), np.float32)
    Sg = np.zeros((rot, CTX + S), np.float32)
    C[:, CTX:] = np.concatenate([cs, cs], -1).T
    Sg[:, CTX:] = np.concatenate([-sn, sn], -1).T
    return np.ascontiguousarray(C), np.ascontiguousarray(Sg)


def fm(v, n=128):
    return np.ascontiguousarray(np.asarray(v, np.float32).reshape(-1, n).T)


def prep_inputs(inp, c):
    D, KC, H, NCORE, CPB = c['D'], c['KC'], c['HPC'], c['NCORE'], c['CPB']
    TL, TC, QL, KVL, NH, BW, NE, DE, G = c['TL'], c['TC'], c['QL'], c['KVL'], c['NH'], c['BW'], c['NE'], c['DE'], c['G']
    WH, WW, GW = c['WH'], c['WW'], c['GRID_W']
    g = {k_: np.asarray(v) for k_, v in inp.items()}
    cvec = np.stack([g['c'][0], g['c'][1], g['c_ctx']]).astype(np.float32)
    cT = np.ascontiguousarray(cvec.reshape(3, KC, 128).transpose(2, 1, 0).reshape(128, KC * 3))
    wr = np.ascontiguousarray(g['w_router'].reshape(KC, 128, NE).transpose(1, 0, 2).reshape(128, KC * NE))
    c64, s64 = rope_tables(c, 64)
    c128, s128 = rope_tables(c, 128)
    ident = np.eye(128, dtype=np.float32)
    o_cq = 0; o_ckv = QL; o_kr = QL + KVL; o_gq = o_kr + 64; o_gk = o_gq + NH * 128; o_gv = o_gk + 256
    o_nq = o_gv + 256; o_nk = o_nq + NH * 128; o_nv = o_nk + NH * 128; o_lx = o_nv + NH * 128; o_lg = o_lx + BW
    sw64 = np.concatenate([np.arange(32, 64), np.arange(0, 32)])
    sw128 = np.concatenate([np.arange(64, 128), np.arange(0, 64)])
    p = np.arange(128)[:, None, None]
    i4 = np.arange(4)[None, :, None]
    qc = np.arange(64)[None, None, :]
    t = i4 * 128 + p
    wi = t // 64
    col = t % 64
    cs = np.clip(qc - WW // 2, 0, GW - WW)
    ok = (col >= cs) & (col < cs + WW)
    dc = np.clip(col - qc + (WW - 1), 0, 2 * WW - 2)
    maps = []
    for core in range(NCORE):
        b, j = core // CPB, core % CPB
        m = {'x_own': g['x'][b, TL * j:TL * (j + 1)], 'ctx_own': g['ctx'][b, TC * j:TC * (j + 1)], 'cT': cT,
             'ident': ident, 'wr': wr, 'rb': g['router_bias'][None, :], 'fnorm': g['final_norm'][None, :],
             'c64': c64, 's64': s64, 'c128': c128, 's128': s128}
        sel = np.zeros((3, 2), np.float32); sel[b, 0] = 1; sel[2, 1] = 1
        oh = np.zeros((128, CPB), np.float32); oh[:, j] = 1; m['oh'] = oh
        ohb = np.zeros((128, c['B']), np.float32); ohb[:, b] = 1; m['ohb'] = ohb
        m['sel'] = sel
        heads = [j * H + hh for hh in range(H)]
        kvh = (j * H) // G
        for l in range(c['DEPTH']):
            MS = c['MS']
            m[f'wmod{l}'] = g['w_mod'][l][:, MS * core:MS * (core + 1)]
            m[f'bmod{l}'] = g['b_mod'][l][None, MS * core:MS * (core + 1)]
            cd = {'cq': [np.arange(o_cq, o_cq + QL)], 'ckv': [np.arange(o_ckv, o_ckv + KVL)], 'kr': [o_kr + np.arange(64)], 'krs': [o_kr + sw64],
                  'gq': [o_gq + h * 128 + np.arange(128) for h in heads], 'gqs': [o_gq + h * 128 + sw128 for h in heads],
                  'gk': [o_gk + kvh * 128 + np.arange(128)], 'gks': [o_gk + kvh * 128 + sw128], 'gv': [o_gv + kvh * 128 + np.arange(128)]}
            for nm_, o in (('nq', o_nq), ('nk', o_nk), ('nv', o_nv), ('lx', o_lx), ('lg', o_lg)):
                cd[nm_] = [o + h * 128 + np.arange(128) for h in heads]
            cols = []
            for nm_ in c['SEG']:
                cols += cd[nm_]
            m[f'wpc{l}'] = g['w_in'][l][:, np.concatenate(cols)]

            def shard(a):
                R = a.shape[0] // NCORE
                return a[R * core:R * (core + 1)]
            m[f'wbg{l}'] = shard(g['w_branch_gate'][l].reshape(4 * D, D))
            m[f'wb{l}'] = shard(g['w_branch'][l].reshape(4 * BW, D))
            m[f'wout{l}'] = shard(g['w_out'][l])
            m[f'weg{l}'] = shard(g['w_exp_gate'][l].reshape(NE * D, DE))
            m[f'weu{l}'] = shard(g['w_exp_up'][l].reshape(NE * D, DE))
            m[f'wed{l}'] = shard(g['w_exp_down'][l].reshape(NE * DE, D))
            m[f'nmix{l}'] = fm(g['norm_mix'][l]); m[f'nffn{l}'] = fm(g['norm_ffn'][l])
            m[f'qn{l}'] = fm(g['mla_q_norm'][l]); m[f'kvn{l}'] = fm(g['mla_kv_norm'][l])
            uq = []
            for h in heads:
                uq += [h * 192 + np.arange(128), h * 192 + 128 + np.arange(64), h * 192 + 128 + sw64]
            m[f'wuq{l}'] = g['mla_w_uq'][l][:, np.concatenate(uq)]
            m[f'wukv{l}'] = g['mla_w_ukv'][l][:, np.concatenate([h * 256 + np.arange(256) for h in heads])]
            m[f'gqn{l}'] = np.stack([g['gqa_q_norm'][l], g['gqa_q_norm'][l][sw128]], 1)
            m[f'gkn{l}'] = np.stack([g['gqa_k_norm'][l], g['gqa_k_norm'][l][sw128]], 1)
            nab = np.empty((H, WH, 128, 4, 64), np.float32)
            for hh, h in enumerate(heads):
                for v in range(WH):
                    dr = np.clip(wi - v + (WH - 1), 0, 2 * WH - 2)
                    vals = g['na_rpb'][l][h][np.broadcast_to(dr, ok.shape), np.broadcast_to(dc, ok.shape)]
                    nab[hh, v] = np.where(ok, vals, np.float32(NEG))
            m[f'nab{l}'] = nab.reshape(H * WH * 128, 256)
            chs = [np.arange(h * 128, h * 128 + 128) for h in heads]
            m[f'lcw{l}'] = np.concatenate([g['lru_conv_w'][l][:, ch].T for ch in chs], 1)
            m[f'lcb{l}'] = np.stack([g['lru_conv_b'][l][ch] for ch in chs], 1)
            m[f'lwa{l}'] = np.concatenate([g['lru_w_a'][l][d, h] for d in range(2) for h in heads], 1)
            m[f'lwx{l}'] = np.concatenate([g['lru_w_x'][l][d, h] for d in range(2) for h in heads], 1)
            for nm, src in (('lba', 'lru_b_a'), ('lbx', 'lru_b_x'), ('llam', 'lru_lambda')):
                m[f'{nm}{l}'] = np.stack([g[src][l][d][ch] for d in range(2) for ch in chs], 1)
            m[f'bbg{l}'] = np.concatenate([fm(g['b_branch_gate'][l][i]) for i in range(4)], 1)
        maps.append({k_: np.ascontiguousarray(v, dtype=np.float32) for k_, v in m.items()})
    return maps


_CACHE = {}


def kernel(**inputs):
    c = derive(FULL)
    k = build(c)
    k.stages['all']()
    maps = prep_inputs(inputs, c)
    maps = [{n: v for n, v in m.items() if n in k.I} for m in maps]
    res = run_bass_kernel_spmd(k.nc, maps, core_ids=list(range(c['NCORE'])))
    out = np.empty((c['B'], c['S'], c['D']), np.float32)
    for core in range(c['NCORE']):
        b, j = core // c['CPB'], core % c['CPB']
        out[b, c['TL'] * j:c['TL'] * (j + 1)] = np.asarray(res.results[core]['out_own'])
    return out
```
